# Optimizing a Trainium2 kernel written in Bass

```python
import jax, jax.numpy as jnp
from jax import lax
import numpy as np

D_MODEL = 1024
BATCH = 16
SEQ = 2048
DEPTH = 4

HEAD_DIM = 64
N_HEADS = D_MODEL // 256
BRANCH_WIDTH = N_HEADS * HEAD_DIM
N_BRANCHES = 4
ROT_DIM = HEAD_DIM // 4
ROPE_THETA = 500000.0
Q_BLOCK = 128
DILATED_PAIRS = ((128, 1), (512, 4), (2048, 16))
WIN_BLOCK = 128
MOBA_BLOCK = 256
MOBA_TOPK = 3
MOBA_Q_CHUNK = 32
MLA_Q_RANK = D_MODEL // 4
MLA_KV_RANK = D_MODEL // 8
MLA_NOPE_DIM = 64
MLA_ROPE_DIM = 32
MLA_V_DIM = 64
D_FF = 4 * D_MODEL
NORM_EPS = 1e-6
FORGET_BIAS_INIT = 3.0
IN_SIZES = (3 * BRANCH_WIDTH, N_HEADS, 3 * BRANCH_WIDTH, 3 * BRANCH_WIDTH,
            MLA_Q_RANK, MLA_KV_RANK, MLA_ROPE_DIM, N_BRANCHES * D_MODEL)
D_IN = sum(IN_SIZES)

kernel_name = "hybrid_fox_dilated_moba_mla_block"


def rms_norm(x, g):
    xf = x.astype(jnp.float32)
    y = xf * lax.rsqrt(jnp.mean(jnp.square(xf), axis=-1, keepdims=True) + NORM_EPS)
    return (y * g.astype(jnp.float32)).astype(x.dtype)


def rope_tables(seq, dim):
    inv_freq = 1.0 / (ROPE_THETA ** (jnp.arange(0, dim, 2, dtype=jnp.float32) / dim))
    ang = jnp.arange(seq, dtype=jnp.float32)[:, None] * inv_freq[None, :]
    return jnp.cos(ang), jnp.sin(ang)


def apply_rope(x, cos, sin):
    xf = x.astype(jnp.float32)
    x1, x2 = jnp.split(xf, 2, axis=-1)
    return jnp.concatenate([x1 * cos - x2 * sin, x2 * cos + x1 * sin], axis=-1).astype(x.dtype)


def partial_rope(x, cos, sin):
    return jnp.concatenate([apply_rope(x[..., :ROT_DIM], cos, sin), x[..., ROT_DIM:]], axis=-1)


def to_heads(t):
    b, s, c = t.shape
    return t.reshape(b, s, N_HEADS, c // N_HEADS).transpose(0, 2, 1, 3)


def from_heads(t):
    b, h, s, d = t.shape
    return t.transpose(0, 2, 1, 3).reshape(b, s, h * d)


def blocked_causal_attention(q, k, v, scale, cum=None):
    b, h, s, dk = q.shape
    nq = s // Q_BLOCK
    qb = q.reshape(b, h, nq, Q_BLOCK, dk).transpose(2, 0, 1, 3, 4)
    kpos = jnp.arange(s)
    xs = (jnp.arange(nq), qb)
    if cum is not None:
        xs = xs + (cum.reshape(b, h, nq, Q_BLOCK).transpose(2, 0, 1, 3),)

    def one_block(args):
        qpos = args[0] * Q_BLOCK + jnp.arange(Q_BLOCK)
        sc = jnp.einsum('bhqd,bhkd->bhqk', args[1], k).astype(jnp.float32) * scale
        if cum is not None:
            sc = sc + args[2][..., :, None] - cum[:, :, None, :]
        sc = jnp.where(kpos[None, :] <= qpos[:, None], sc, -jnp.inf)
        p = jax.nn.softmax(sc, axis=-1).astype(v.dtype)
        return jnp.einsum('bhqk,bhkd->bhqd', p, v)

    out = lax.map(one_block, xs)
    return out.transpose(1, 2, 0, 3, 4).reshape(b, h, s, v.shape[-1])


def banded_window_attention(q, k, v, window, scale):
    n, length, dh = q.shape
    lp = -(-length // WIN_BLOCK) * WIN_BLOCK
    pad = ((0, 0), (0, lp - length), (0, 0))
    nb = lp // WIN_BLOCK
    qb, kb, vb = (jnp.pad(t, pad).reshape(n, nb, WIN_BLOCK, dh) for t in (q, k, v))

    def with_prev(t):
        prev = jnp.pad(t, ((0, 0), (1, 0), (0, 0), (0, 0)))[:, :-1]
        return jnp.concatenate([prev, t], axis=2)

    k2, v2 = with_prev(kb), with_prev(vb)
    sc = jnp.einsum('nbqd,nbkd->nbqk', qb, k2).astype(jnp.float32) * scale
    qi = jnp.arange(WIN_BLOCK)[:, None] + WIN_BLOCK
    kj = jnp.arange(2 * WIN_BLOCK)[None, :]
    dist = qi - kj
    band = (dist >= 0) & (dist <= window)
    real = (jnp.arange(nb) > 0)[:, None, None] | (kj >= WIN_BLOCK)[None]
    sc = jnp.where(band[None] & real, sc, -jnp.inf)
    lse = jax.nn.logsumexp(sc, axis=-1)
    p = jnp.exp(sc - lse[..., None]).astype(v.dtype)
    out = jnp.einsum('nbqk,nbkd->nbqd', p, v2).reshape(n, lp, dh)[:, :length]
    return out, lse.reshape(n, lp)[:, :length]


def dilated_attention(q, k, v):
    b, h, s, dh = q.shape
    scale = dh ** -0.5
    outs, lses = [], []
    for window, dil in DILATED_PAIRS:
        sub = s // dil

        def to_sub(t):
            return t.reshape(b, h, sub, dil, dh).transpose(0, 1, 3, 2, 4).reshape(b * h * dil, sub, dh)

        o, l = banded_window_attention(to_sub(q), to_sub(k), to_sub(v), window // dil, scale)
        outs.append(o.reshape(b, h, dil, sub, dh).transpose(0, 1, 3, 2, 4).reshape(b, h, s, dh))
        lses.append(l.reshape(b, h, dil, sub).transpose(0, 1, 3, 2).reshape(b, h, s))
    wts = jax.nn.softmax(jnp.stack(lses, axis=0), axis=0)
    return jnp.sum(wts[..., None].astype(v.dtype) * jnp.stack(outs, axis=0), axis=0)


def moba_attention(q, k, v):
    b, h, s, dh = q.shape
    scale = dh ** -0.5
    sp = -(-s // MOBA_BLOCK) * MOBA_BLOCK
    padw = ((0, 0), (0, 0), (0, sp - s), (0, 0))
    q, k, v = jnp.pad(q, padw), jnp.pad(k, padw), jnp.pad(v, padw)
    nblk = sp // MOBA_BLOCK
    n_sel = min(MOBA_TOPK, nblk - 1)
    kb = k.reshape(b, h, nblk, MOBA_BLOCK, dh)
    vb = v.reshape(b, h, nblk, MOBA_BLOCK, dh)
    nc = sp // MOBA_Q_CHUNK
    xs = (jnp.arange(nc), q.reshape(b, h, nc, MOBA_Q_CHUNK, dh).transpose(2, 0, 1, 3, 4))
    if n_sel > 0:
        kmean = jnp.mean(kb.astype(jnp.float32), axis=3)
        gate = jnp.einsum('bhsd,bhnd->bhsn', q.astype(jnp.float32), kmean)
        past = jnp.arange(nblk)[None, :] < (jnp.arange(sp) // MOBA_BLOCK)[:, None]
        _, sel = lax.top_k(jnp.where(past, gate, -jnp.inf), n_sel)
        xs = xs + (sel.reshape(b, h, nc, MOBA_Q_CHUNK, n_sel).transpose(2, 0, 1, 3, 4),)
    bi = jnp.arange(b)[:, None, None, None]
    hi = jnp.arange(h)[None, :, None, None]

    def one_chunk(args):
        i, qi = args[0], args[1]
        qpos = i * MOBA_Q_CHUNK + jnp.arange(MOBA_Q_CHUNK)
        own = (i * MOBA_Q_CHUNK) // MOBA_BLOCK
        k_own = lax.dynamic_index_in_dim(kb, own, axis=2, keepdims=False)
        v_own = lax.dynamic_index_in_dim(vb, own, axis=2, keepdims=False)
        kpos = own * MOBA_BLOCK + jnp.arange(MOBA_BLOCK)
        s_own = jnp.einsum('bhqd,bhkd->bhqk', qi, k_own).astype(jnp.float32) * scale
        s_own = jnp.where(kpos[None, :] <= qpos[:, None], s_own, -jnp.inf)
        if n_sel == 0:
            p = jax.nn.softmax(s_own, axis=-1).astype(v.dtype)
            return jnp.einsum('bhqk,bhkd->bhqd', p, v_own)
        seli = args[2]
        k_sel = kb[bi, hi, seli]
        v_sel = vb[bi, hi, seli]
        s_sel = jnp.einsum('bhqd,bhqnkd->bhqnk', qi, k_sel).astype(jnp.float32) * scale
        valid = jnp.arange(n_sel)[None, :] < (qpos // MOBA_BLOCK)[:, None]
        s_sel = jnp.where(valid[:, :, None], s_sel, -jnp.inf)
        s_sel = s_sel.reshape(b, h, MOBA_Q_CHUNK, n_sel * MOBA_BLOCK)
        p = jax.nn.softmax(jnp.concatenate([s_sel, s_own], axis=-1), axis=-1)
        p_sel = p[..., :n_sel * MOBA_BLOCK].reshape(b, h, MOBA_Q_CHUNK, n_sel, MOBA_BLOCK).astype(v.dtype)
        p_own = p[..., n_sel * MOBA_BLOCK:].astype(v.dtype)
        return (jnp.einsum('bhqnk,bhqnkd->bhqd', p_sel, v_sel)
                + jnp.einsum('bhqk,bhkd->bhqd', p_own, v_own))

    out = lax.map(one_chunk, xs)
    return out.transpose(1, 2, 0, 3, 4).reshape(b, h, sp, dh)[:, :, :s]


def mla_attention(cq_raw, ckv_raw, kr_raw, g_cq, g_ckv, w_uq, w_uk, w_uv, cos_m, sin_m):
    b, s, _ = cq_raw.shape
    c_q = rms_norm(cq_raw, g_cq)
    c_kv = rms_norm(ckv_raw, g_ckv)
    q = to_heads(c_q @ w_uq)
    q = jnp.concatenate([q[..., :MLA_NOPE_DIM], apply_rope(q[..., MLA_NOPE_DIM:], cos_m, sin_m)], axis=-1)
    k_nope = to_heads(c_kv @ w_uk)
    v = to_heads(c_kv @ w_uv)
    k_rope = apply_rope(kr_raw[:, None], cos_m, sin_m)
    k = jnp.concatenate([k_nope, jnp.broadcast_to(k_rope, (b, N_HEADS, s, MLA_ROPE_DIM))], axis=-1)
    return blocked_causal_attention(q, k, v, (MLA_NOPE_DIM + MLA_ROPE_DIM) ** -0.5)


def hybrid_mixer(h, w_in, b_forget, g_cq, g_ckv, w_uq, w_uk, w_uv, w_branch, w_out,
                 cos_p, sin_p, cos_m, sin_m):
    b, s, d = h.shape
    proj = h @ w_in
    offs = np.cumsum(IN_SIZES)[:-1].tolist()
    fox_qkv, fox_f, dil_qkv, moba_qkv, mla_cq, mla_ckv, mla_kr, gates = jnp.split(proj, offs, axis=-1)

    q, k, v = (to_heads(t) for t in jnp.split(fox_qkv, 3, axis=-1))
    log_f = jax.nn.log_sigmoid((fox_f + b_forget).astype(jnp.float32))
    cum = jnp.cumsum(log_f, axis=1).transpose(0, 2, 1)
    y_fox = blocked_causal_attention(q, k, v, HEAD_DIM ** -0.5, cum)

    q, k, v = (to_heads(t) for t in jnp.split(dil_qkv, 3, axis=-1))
    y_dil = dilated_attention(partial_rope(q, cos_p, sin_p), partial_rope(k, cos_p, sin_p), v)

    q, k, v = (to_heads(t) for t in jnp.split(moba_qkv, 3, axis=-1))
    y_moba = moba_attention(partial_rope(q, cos_p, sin_p), partial_rope(k, cos_p, sin_p), v)

    y_mla = mla_attention(mla_cq, mla_ckv, mla_kr, g_cq, g_ckv, w_uq, w_uk, w_uv, cos_m, sin_m)

    ys = jnp.stack([from_heads(y_fox), from_heads(y_dil), from_heads(y_moba), from_heads(y_mla)], axis=2)
    u = jnp.einsum('bsnc,ncd->bsnd', ys, w_branch)
    g = jax.nn.sigmoid(gates.reshape(b, s, N_BRANCHES, d))
    merged = jnp.sum(g * u, axis=2)
    return merged @ w_out


def squared_relu_mlp(h, w_up, w_down):
    return jnp.square(jax.nn.relu(h @ w_up)) @ w_down


def setup_inputs(seed: int = 0) -> dict:
    key = jax.random.key(seed)
    ks = jax.random.split(key, 16)

    def nrm(k, shape, fan_in):
        return jax.random.normal(k, shape, jnp.float32) * fan_in ** -0.5

    def gain(k, shape):
        return 1.0 + 0.05 * jax.random.normal(k, shape, jnp.float32)

    return {
        "x": jax.random.normal(ks[0], (BATCH, SEQ, D_MODEL), jnp.float32),
        "w_in": nrm(ks[1], (DEPTH, D_MODEL, D_IN), D_MODEL),
        "b_forget": FORGET_BIAS_INIT + 0.5 * jax.random.normal(ks[2], (DEPTH, N_HEADS), jnp.float32),
        "g_cq": gain(ks[3], (DEPTH, MLA_Q_RANK)),
        "g_ckv": gain(ks[4], (DEPTH, MLA_KV_RANK)),
        "w_uq": nrm(ks[5], (DEPTH, MLA_Q_RANK, N_HEADS * (MLA_NOPE_DIM + MLA_ROPE_DIM)), MLA_Q_RANK),
        "w_uk": nrm(ks[6], (DEPTH, MLA_KV_RANK, N_HEADS * MLA_NOPE_DIM), MLA_KV_RANK),
        "w_uv": nrm(ks[7], (DEPTH, MLA_KV_RANK, N_HEADS * MLA_V_DIM), MLA_KV_RANK),
        "w_branch": nrm(ks[8], (DEPTH, N_BRANCHES, BRANCH_WIDTH, D_MODEL), BRANCH_WIDTH),
        "w_out": nrm(ks[9], (DEPTH, D_MODEL, D_MODEL), D_MODEL),
        "w_up": nrm(ks[10], (DEPTH, D_MODEL, D_FF), D_MODEL),
        "w_down": nrm(ks[11], (DEPTH, D_FF, D_MODEL), D_FF),
        "g_pre_mix": gain(ks[12], (DEPTH, D_MODEL)),
        "g_post_mix": gain(ks[13], (DEPTH, D_MODEL)),
        "g_pre_mlp": gain(ks[14], (DEPTH, D_MODEL)),
        "g_post_mlp": gain(ks[15], (DEPTH, D_MODEL)),
    }


def reference(x, w_in, b_forget, g_cq, g_ckv, w_uq, w_uk, w_uv, w_branch, w_out, w_up, w_down,
              g_pre_mix, g_post_mix, g_pre_mlp, g_post_mlp):
    s = x.shape[1]
    cos_p, sin_p = rope_tables(s, ROT_DIM)
    cos_m, sin_m = rope_tables(s, MLA_ROPE_DIM)
    for l in range(DEPTH):
        h = rms_norm(x, g_pre_mix[l])
        mix = hybrid_mixer(h, w_in[l], b_forget[l], g_cq[l], g_ckv[l], w_uq[l], w_uk[l], w_uv[l],
                           w_branch[l], w_out[l], cos_p, sin_p, cos_m, sin_m)
        x = x + rms_norm(mix, g_post_mix[l])
        h = rms_norm(x, g_pre_mlp[l])
        x = x + rms_norm(squared_relu_mlp(h, w_up[l], w_down[l]), g_post_mlp[l])
    return x
```

```python
import contextlib
import numpy as np
import concourse.bass as bass
import concourse.mybir as mybir
from concourse.bass_utils import run_bass_kernel_spmd

F32 = mybir.dt.float32
BF16 = mybir.dt.bfloat16
AF = mybir.ActivationFunctionType
ALU = mybir.AluOpType
AX = mybir.AxisListType

S = 2048
D = 1024
NCORE = 8
DEPTH = 4
NTC = 4
NTB = 16
WSLOT = 4096
NSLOT = 2

C_FQ, C_FK, C_FV, C_FF = 0, 256, 512, 768
C_DQ, C_DK, C_DV = 772, 1028, 1284
C_MQ, C_MK, C_MV = 1540, 1796, 2052
C_CQ, C_CKV, C_KR, C_G = 2308, 2564, 2692, 2724

COMPUTE = ('pe', 'act', 'dve', 'pool')
NDS = 12


class Op:
    __slots__ = ('eng', 'fn', 'deps', 'dma', 'idx', 'signal', 'count', 'waits', 'dma_id')

    def __init__(self, eng, fn, dma):
        self.eng = eng
        self.fn = fn
        self.dma = dma
        self.deps = []
        self.signal = False
        self.count = 0
        self.waits = []
        self.dma_id = -1


class Prog:
    def __init__(self, nc):
        self.nc = nc
        self.ops = []
        self.last_w = {}
        self.readers = {}
        self.n_dma = [0, 0]

    def op(self, eng, fn, r=(), w=(), dma=False):
        o = Op(eng, fn, dma)
        o.idx = len(self.ops)
        deps = {}
        for k in r:
            p = self.last_w.get(k)
            if p is not None:
                deps[p] = 'raw'
        for k in w:
            p = self.last_w.get(k)
            if p is not None and p not in deps:
                deps[p] = 'waw'
            for rd in self.readers.get(k, ()):
                if rd not in deps:
                    deps[rd] = 'war'
        if dma:
            cls = 1 if eng == 'pool' else 0
            o.dma_id = (cls, self.n_dma[cls])
            self.n_dma[cls] += 1
        o.deps = list(deps.items())
        for k in w:
            self.last_w[k] = o.idx
            self.readers[k] = []
        for k in r:
            lst = self.readers.setdefault(k, [])
            if not dma:
                lst[:] = [x for x in lst if self.ops[x].dma or self.ops[x].eng != eng]
            lst.append(o.idx)
        self.ops.append(o)
        return o

    @staticmethod
    def _needs_wait(cons, prod, kind):
        if prod.dma or cons.dma:
            return True
        if prod.eng != cons.eng:
            return True
        if cons.eng == 'pe':
            return False
        return True

    def finalize(self):
        ops = self.ops
        for o in ops:
            for (p, kind) in o.deps:
                po = ops[p]
                if self._needs_wait(o, po, kind) and not po.dma:
                    po.signal = True
        cnt = {e: 0 for e in COMPUTE}
        for o in ops:
            if (not o.dma) and o.signal:
                cnt[o.eng] += 1
                o.count = cnt[o.eng]
        waited = {}
        waited_dma = {}
        for o in ops:
            need = {}
            need_dma = {}
            for (p, kind) in o.deps:
                po = ops[p]
                if not self._needs_wait(o, po, kind):
                    continue
                if po.dma:
                    need_dma[po.dma_id] = True
                else:
                    need[po.eng] = max(need.get(po.eng, 0), po.count)
            if o.dma and o.dma_id[1] >= NDS:
                need_dma[(o.dma_id[0], o.dma_id[1] - NDS)] = True
            w = []
            for e, c in need.items():
                if waited.get((o.eng, e), 0) < c:
                    waited[(o.eng, e)] = c
                    w.append(('c', e, c))
            for d in need_dma:
                s = d[0] * NDS + d[1] % NDS
                tgt = 16 * (d[1] // NDS + 1)
                if waited_dma.get((o.eng, s), 0) < tgt:
                    waited_dma[(o.eng, s)] = tgt
                    w.append(('d', s, tgt))
            o.waits = w
        self.final_counts = cnt

    def emit(self):
        nc = self.nc
        self.finalize()
        with contextlib.ExitStack() as es:
            csem = {e: es.enter_context(nc.semaphore('cs_' + e)) for e in COMPUTE}
            dsem = [es.enter_context(nc.semaphore('ds_%d' % i)) for i in range(2 * NDS)]
            block = es.enter_context(nc.Block())
            by_eng = {}
            for o in self.ops:
                by_eng.setdefault(o.eng, []).append(o)
            ndma = self.n_dma

            def run(ename, e):
                for o in by_eng.get(ename, []):
                    for (t, a, b) in o.waits:
                        if t == 'c':
                            e.wait_ge(csem[a], b)
                        else:
                            e.wait_ge(dsem[a], b)
                    ins = o.fn(e)
                    if o.dma:
                        ins.then_inc(dsem[o.dma_id[0] * NDS + o.dma_id[1] % NDS], 16)
                    elif o.signal:
                        ins.then_inc(csem[o.eng], 1)
                if ename == 'sp':
                    for cls in range(2):
                        for s in range(NDS):
                            n = (ndma[cls] - s + NDS - 1) // NDS if ndma[cls] > s else 0
                            if n > 0:
                                e.wait_ge(dsem[cls * NDS + s], 16 * n)
                    for ce in COMPUTE:
                        if self.final_counts[ce] > 0:
                            e.wait_ge(csem[ce], self.final_counts[ce])

            @block.tensor
            def _(e):
                run('pe', e)

            @block.scalar
            def _(e):
                run('act', e)

            @block.vector
            def _(e):
                run('dve', e)

            @block.gpsimd
            def _(e):
                run('pool', e)

            @block.sync
            def _(e):
                run('sp', e)


def _st(w2d, cols):
    K = w2d.shape[0]
    nk = K // 128
    cols = np.asarray(cols, dtype=np.int64)
    wp = np.concatenate([w2d, np.zeros((K, 1), w2d.dtype)], axis=1)
    g = wp[:, cols]
    M = len(cols)
    return np.ascontiguousarray(g.reshape(nk, 128, M).transpose(1, 0, 2)).reshape(128, nk * M)


def _rope8_cols(base, h, kind):
    if kind == 'q':
        feat = [0, 0, 1, 1, 0, 0, 1, 1]
    else:
        feat = [0, 0, 1, 1, 1, 1, 0, 0]
    cols = []
    for e in range(8):
        for f in range(8):
            cols.append(base + h * 64 + feat[e] * 8 + f)
    for d in range(16, 64):
        cols.append(base + h * 64 + d)
    return cols


def unit_defs():
    U = {}

    U['mla_lat'] = (8 * 384, lambda W: _st(W['w_in'], list(range(C_CQ, C_CQ + 256)) + list(range(C_CKV, C_CKV + 128))))

    def kr_cols():
        p1, p2 = [-1] * 64, [-1] * 64
        f1 = [0, 1, 1, 0]
        f2 = [1, 0, 0, 1]
        for e in range(4):
            for f in range(16):
                p1.append(C_KR + f1[e] * 16 + f)
        for e in range(4):
            for f in range(16):
                p2.append(C_KR + f2[e] * 16 + f)
        return p1 + p2
    U['mla_kr'] = (8 * 256, lambda W: _st(W['w_in'], kr_cols()))

    def mla_up(W):
        a = _st(W['w_uk'], list(range(256)))
        b = _st(W['w_uv'], list(range(256)))
        cols = []
        fq = [0, 0, 1, 1]
        for h in range(4):
            for d in range(64):
                cols.append(h * 96 + d)
            for e in range(4):
                for f in range(16):
                    cols.append(h * 96 + 64 + fq[e] * 16 + f)
        c = _st(W['w_uq'], cols)
        return np.concatenate([a, b, c], axis=1)
    U['mla_up'] = (512 + 1024, mla_up)

    for nm, cq, ck, cv in (('dil', C_DQ, C_DK, C_DV), ('mob', C_MQ, C_MK, C_MV)):
        U[nm + '_v'] = (8 * 256, (lambda W, cv=cv: _st(W['w_in'], list(range(cv, cv + 256)))))
        for h in range(4):
            U['%s_h%d' % (nm, h)] = (8 * 224, (lambda W, cq=cq, ck=ck, h=h:
                                                _st(W['w_in'], _rope8_cols(cq, h, 'q') + _rope8_cols(ck, h, 'k'))))

    def fox_f(W):
        cols = []
        for h in range(4):
            cols += [C_FF + h] * 4
        return _st(W['w_in'], cols)
    U['fox_f'] = (8 * 16, fox_f)
    U['fox_v'] = (8 * 256, lambda W: _st(W['w_in'], list(range(C_FV, C_FV + 256))))
    for h in range(4):
        U['fox_h%d' % h] = (8 * 128, (lambda W, h=h: _st(W['w_in'], list(range(C_FQ + h * 64, C_FQ + h * 64 + 64)) +
                                                        list(range(C_FK + h * 64, C_FK + h * 64 + 64)))))

    for d in range(8):
        for npair in range(2):
            def mg(W, d=d, npair=npair):
                parts = []
                for n in (2 * npair, 2 * npair + 1):
                    parts.append(_st(W['w_in'], list(range(C_G + n * 1024 + d * 128, C_G + n * 1024 + d * 128 + 128))))
                    parts.append(_st(W['w_branch'][n], list(range(d * 128, d * 128 + 128))))
                return np.concatenate(parts, axis=1)
            U['mg_%d_%d' % (d, npair)] = (2 * 1280, mg)
    for u in range(2):
        U['wo_%d' % u] = (8 * 512, (lambda W, u=u: _st(W['w_out'], list(range(u * 512, u * 512 + 512)))))
    for u in range(8):
        U['up_%d' % u] = (8 * 512, (lambda W, u=u: _st(W['w_up'], list(range(u * 512, u * 512 + 512)))))
    for fh in range(2):
        for q in range(4):
            U['dn_%d_%d' % (fh, q)] = (16 * 256, (lambda W, fh=fh, q=q: _st(W['w_down'][fh * 2048:(fh + 1) * 2048],
                                                                          list(range(q * 256, q * 256 + 256)))))
    return U


def unit_order():
    o = ['mla_lat', 'mla_kr', 'mla_up']
    for nm in ('dil', 'mob'):
        o += [nm + '_h0', nm + '_v', nm + '_h1', nm + '_h2', nm + '_h3']
    o += ['fox_f', 'fox_h0', 'fox_v', 'fox_h1', 'fox_h2', 'fox_h3']
    for hf in range(2):
        for d in range(8):
            for npair in range(2):
                o.append('mg_%d_%d' % (d, npair))
    o += ['wo_0', 'wo_1']
    for th in range(2):
        for fh in range(2):
            o += ['up_%d' % u for u in range(4 * fh, 4 * fh + 4)]
            o += ['dn_%d_%d' % (fh, q) for q in range(4)]
    return o


UNITS = unit_defs()
UOFF = {}
_off = 0
for _k, (_f, _) in UNITS.items():
    UOFF[_k] = _off
    _off += _f
FTOT = _off

SM_GPM, SM_GQM, SM_GPL, SM_GQL = 0, 32, 64, 96
SM_GCQ, SM_GCKV, SM_BF, SM_AUG, SM_EPS, SM_NEG, SM_VAL, SM_ONE = 128, 136, 140, 156, 162, 163, 227, 291
NSM = 292

CB_ROPE = 0
CB_DM = 3 * 2048
CB_CM = CB_DM + 9 * 128
CB_ID = CB_CM + 512
CB_ONE = CB_ID + 128
NCB = CB_ONE + 512


def build_consts():
    cb = np.zeros((128, NCB), np.float32)
    t = np.arange(S, dtype=np.float64)
    invf = 1.0 / (500000.0 ** (np.arange(0, 16, 2, dtype=np.float64) / 16.0))
    ang = np.float32(t)[None, :].astype(np.float64) * np.float32(invf)[:, None].astype(np.float64)
    c, s = np.cos(ang), np.sin(ang)
    tq = [c, s, c, s, s, c, s, c]
    tk = [c, s, c, s, c, -s, -c, s]
    A = cb[:, CB_ROPE:CB_ROPE + 2048]
    B = cb[:, CB_ROPE + 2048:CB_ROPE + 4096]
    Cc = cb[:, CB_ROPE + 4096:CB_ROPE + 6144]
    for e in range(8):
        A[8 * e:8 * e + 8] = 0.125 * tq[e]
        B[8 * e:8 * e + 8] = tk[e]
    invm = 1.0 / (500000.0 ** (np.arange(0, 32, 2, dtype=np.float64) / 32.0))
    angm = np.float32(t)[None, :].astype(np.float64) * np.float32(invm)[:, None].astype(np.float64)
    cm, sm = np.cos(angm), np.sin(angm)
    sc = 96.0 ** -0.5
    tq4 = [cm, sm, cm, sm]
    ta = [cm, cm, cm, -cm]
    tb = [-sm, sm, sm, sm]
    for e in range(4):
        A[64 + 16 * e:64 + 16 * e + 16] = sc * tq4[e]
        B[64 + 16 * e:64 + 16 * e + 16] = ta[e]
        Cc[64 + 16 * e:64 + 16 * e + 16] = tb[e]
    sk = np.arange(128)[:, None]
    tq_ = np.arange(128)[None, :]
    for dl in range(9):
        dist = 128 * dl + tq_ - sk
        m = ((dist >= 0) & (dist <= 128)).astype(np.float32) + ((dist >= 0) & (dist % 4 == 0) & (dist <= 512)) + \
            ((dist >= 0) & (dist % 16 == 0))
        cb[:, CB_DM + 128 * dl:CB_DM + 128 * dl + 128] = m
    cmk = np.ones((128, 512), np.float32)
    cmk[:, 0:128] = (tq_ >= sk)
    cb[:, CB_CM:CB_CM + 512] = cmk
    cb[:, CB_ID:CB_ID + 128] = np.eye(128, dtype=np.float32)
    cb[:, CB_ONE:CB_ONE + 512] = 1.0
    ind = np.zeros((8, S), np.float32)
    for n in range(8):
        ind[n, 256 * n:256 * n + 256] = 1.0
    return cb, ind


def build_small(inp):
    sm = np.zeros((128, NSM), np.float32)
    for l in range(DEPTH):
        for nm, off in (('g_pre_mix', SM_GPM), ('g_post_mix', SM_GQM), ('g_pre_mlp', SM_GPL), ('g_post_mlp', SM_GQL)):
            sm[:, off + 8 * l:off + 8 * l + 8] = inp[nm][l].reshape(8, 128).T
        sm[:, SM_GCQ + 2 * l:SM_GCQ + 2 * l + 2] = inp['g_cq'][l].reshape(2, 128).T
        sm[:, SM_GCKV + l] = inp['g_ckv'][l]
        for h in range(4):
            sm[0:4, SM_BF + 4 * l + h] = inp['b_forget'][l, h]
    aug = np.array([[-1, 0, 0, 0], [0, -1, 0, 0], [0, 0, 1, 1], [0, 0, 1, 0], [0, 0, 0, 1], [1, 1, 0, 0]], np.float32).T
    sm[0:4, SM_AUG:SM_AUG + 6] = aug
    sm[:, SM_EPS] = 1e-6
    sm[:, SM_ONE] = 1.0
    for tbi in range(8):
        b = (8 + tbi) // 2
        for n in range(8):
            sm[:, SM_NEG + 8 * tbi + n] = 0.0 if n < b else -1e30
            sm[:, SM_VAL + 8 * tbi + n] = 1.0 if n < b else 0.0
    return sm


def build_weights(inp, nlayers):
    out = np.zeros((nlayers, 128, FTOT), np.float32)
    for l in range(nlayers):
        W = {k: np.asarray(inp[k][l]) for k in ('w_in', 'w_uq', 'w_uk', 'w_uv', 'w_branch', 'w_out', 'w_up', 'w_down')}
        for k, (f, fill) in UNITS.items():
            a = fill(W)
            assert a.shape == (128, f), (k, a.shape, f)
            out[l, :, UOFF[k]:UOFF[k] + f] = a
    return out


STOP = 99
SQENG = 'pool'
EVAC_DVE = False
RS_ALT = False
PN_INTER = False
EVAC2_DVE = False
NPT = 3
MASK_ALT = False
EARLY_PN = True


def build_program(nseq, nlayers, debug=False, phases='padmfgol'):
    nc = bass.Bass("TRN2", target_bir_lowering=False)
    xin = nc.dram_tensor("xT", [nseq, 128, 8 * S], F32, kind="ExternalInput").ap()
    wts = nc.dram_tensor("wts", [nlayers, 128, FTOT], F32, kind="ExternalInput").ap()
    smd = nc.dram_tensor("small", [128, NSM], F32, kind="ExternalInput").ap()
    cbd = nc.dram_tensor("cb", [128, NCB], F32, kind="ExternalInput").ap()
    indd = nc.dram_tensor("ind", [8, S], F32, kind="ExternalInput").ap()
    yout = nc.dram_tensor("yT", [nseq, 128, 8 * S], F32, kind="ExternalOutput").ap()
    if debug:
        dbg = nc.dram_tensor("dbg", [128, 8 * S], F32, kind="ExternalOutput").ap()

    es = contextlib.ExitStack()
    with es:
        def sb(name, shape, dt):
            return es.enter_context(nc.sbuf_tensor(name, shape, dt))

        def pt(name, shape, dt):
            return es.enter_context(nc.psum_tensor(name, shape, dt))

        xT = sb("xT_sb", [128, 8 * S], F32)
        hT = sb("hT", [128, 8 * S], BF16)
        YT = sb("YT", [128, 8 * S], BF16)
        PH = sb("PH", [128, 14400], BF16)
        QKh = PH[:, 0:8192]
        Vt = PH[:, 8192:12352]
        Ytok = PH[:, 12352:14400]
        WR = sb("WR", [128, NSLOT * WSLOT], BF16)
        CB = sb("CB", [128, NCB], BF16)
        SM = sb("SM", [128, NSM], F32)
        PT_ = [sb("PT%d" % i, [128, 512], BF16) for i in range(NPT)]
        T0 = sb("T0", [128, 512], F32)
        T1 = sb("T1", [128, 512], F32)
        B0 = sb("B0", [128, 512], BF16)
        B1 = sb("B1", [128, 512], BF16)
        SMALLT = sb("SMALLT", [128, 256], F32)
        PEN = sb("PEN", [128, 64], BF16)
        KSB = sb("KSB", [128, 16], BF16)
        PENT = sb("PENT", [8, 512], BF16)
        FX = sb("FX", [4, 2 * 512], F32)
        FWT = sb("FWT", [128, 128], BF16)
        FXB = sb("FXB", [4, 512], BF16)

        PJ = [pt("PJ%d" % i, [128, 512], F32) for i in range(2)]
        STp = [pt("ST%d" % i, [128, 512], F32) for i in range(3)]
        OAp = [pt("OA%d" % i, [128, 512], F32) for i in range(2)]
        MS = pt("MS", [128, 512], F32)

        P = Prog(nc)
        cnt = {'pj': 0, 'st': 0, 'oa': 0, 'pt': 0, 'tb': 0, 'sq': 0}

        def rr(name, n):
            v = cnt[name] % n
            cnt[name] += 1
            return v

        def mm(out, lhsT, rhs, start, stop, r, w, sgc=False):
            P.op('pe', lambda e: e.matmul(out, lhsT=lhsT, rhs=rhs, start=start, stop=stop, skip_group_check=sgc), r=r, w=w)

        def act(out, in_, func, r, w, scale=None, bias=None):
            kw = {}
            if scale is not None:
                kw['scale'] = scale
            if bias is not None:
                kw['bias'] = bias
            P.op('act', lambda e: e.activation(out=out, in_=in_, func=func, **kw), r=r, w=w)

        def tt(eng, out, in0, in1, op, r, w):
            P.op(eng, lambda e: e.tensor_tensor(out=out, in0=in0, in1=in1, op=op), r=r, w=w)

        def ts(eng, out, in0, s1, op0, r, w, s2=None, op1=None):
            if op1 is None:
                P.op(eng, lambda e: e.tensor_scalar(out=out, in0=in0, scalar1=s1, scalar2=None, op0=op0), r=r, w=w)
            else:
                P.op(eng, lambda e: e.tensor_scalar(out=out, in0=in0, scalar1=s1, scalar2=s2, op0=op0, op1=op1), r=r, w=w)

        def stt(out, in0, scalar, in1, op0, op1, r, w):
            P.op('dve', lambda e: e.scalar_tensor_tensor(out=out, in0=in0, scalar=scalar, in1=in1, op0=op0, op1=op1),
                 r=r, w=w)

        def cp(eng, out, in_, r, w):
            if eng == 'act':
                P.op('act', lambda e: e.copy(out=out, in_=in_), r=r, w=w)
            else:
                P.op(eng, lambda e: e.tensor_copy(out=out, in_=in_), r=r, w=w)

        def dma(eng, out, in_, r, w):
            P.op(eng, lambda e: e.dma_start(out=out, in_=in_), r=r, w=w, dma=True)

        order_one = unit_order()
        stream = []
        for s_ in range(nseq):
            for l_ in range(nlayers):
                for u in order_one:
                    ph = {'mla': 'a', 'dil': 'd', 'mob': 'm', 'fox': 'f', 'mg_': 'g', 'wo_': 'o', 'up_': 'l', 'dn_': 'l'}[u[:3]]
                    if ph in phases:
                        stream.append((l_, u))
        wstate = {'next_load': 0, 'next_use': 0}

        def w_issue():
            i = wstate['next_load']
            if i >= len(stream):
                return
            l_, u = stream[i]
            f = UNITS[u][0]
            slot = i % NSLOT
            dma('pool', WR[:, slot * WSLOT:slot * WSLOT + f], wts[l_, :, UOFF[u]:UOFF[u] + f], r=[], w=['WR%d' % slot])
            wstate['next_load'] += 1

        def w_get(l_, u):
            i = wstate['next_use']
            assert stream[i] == (l_, u), (stream[i], l_, u)
            while wstate['next_load'] <= i:
                w_issue()
            slot = i % NSLOT
            wstate['next_use'] += 1
            return slot * WSLOT, 'WR%d' % slot

        def w_prefetch():
            while wstate['next_load'] < min(len(stream), wstate['next_use'] + NSLOT - 1):
                w_issue()

        dma('sp', SM[:, :], smd[:, :], r=[], w=['SM'])
        for k in range(0, NCB, 2048):
            k2 = min(NCB, k + 2048)
            dma('pool', CB[:, k:k2], cbd[:, k:k2], r=[], w=['CB'])
        ident = CB[:, CB_ID:CB_ID + 128]
        ones = CB[:, CB_ONE:CB_ONE + 128]
        RS = T1
        GT = SMALLT[:, 0:64]
        TOP8 = SMALLT[:, 64:128]
        RINV = SMALLT[:, 128:132]
        KSF = SMALLT[:, 132:140]
        ONES4 = CB[0:4, CB_ONE:CB_ONE + 512]
        Vv = Vt.rearrange("p (t h d) -> p t h d", t=NTB, h=4)

        def vkeys(tb):
            return ['PV%d' % k for k in range((260 * tb) // 512, (260 * tb + 259) // 512 + 1)]

        def allvkeys():
            return ['PV%d' % k for k in range(9)]

        def qkey(b, tc):
            return 'PH%d' % (8 * b + tc)

        def kkey(b, tc):
            return 'PH%d' % (8 * b + 4 + tc)

        def mxap(c):
            return hT[:, c * S + 1024:c * S + 2048].bitcast(F32)

        def mxkeys(c):
            return ['h%d_2' % c, 'h%d_3' % c]

        def mgap(hf, d, t2):
            if hf == 0:
                return PH[:, d * 1024 + t2 * 512:d * 1024 + t2 * 512 + 512]
            return hT[:, d * S + t2 * 512:d * S + t2 * 512 + 512]

        def mgkey(hf, d, t2):
            if hf == 0:
                return 'PH%d' % (2 * d + t2)
            return 'h%d_%d' % (d, t2)

        def accap(t2):
            return PH[:, 8192 + 1024 * t2:8192 + 1024 * t2 + 1024].bitcast(F32)

        def acckeys(t2):
            return ['PV%d' % (2 * t2), 'PV%d' % (2 * t2 + 1)]

        def v_set_ones():
            P.op('pool', lambda e: e.memset(Vt, 1.0), w=allvkeys())

        def xk(c, tc):
            return 'x%d_%d' % (c, tc)

        def hk(c, tc):
            return 'h%d_%d' % (c, tc)

        def yk(blk):
            return 'Y%d' % blk

        def xs(c, tc):
            return xT[:, c * S + tc * 512:c * S + tc * 512 + 512]

        def hs(c, t0, n):
            return hT[:, c * S + t0:c * S + t0 + n]

        def ys(c, t0, n):
            return YT[:, c * S + t0:c * S + t0 + n]

        rsst = {'i': 0}

        def RSc():
            return (T1 if rsst['i'] % 2 == 1 else T0)[:, :]

        def RSk():
            return 'T1' if rsst['i'] % 2 == 1 else 'T0'

        def rstd_from_ms(nfeat, rkeys):
            if RS_ALT:
                rsst['i'] += 1
            else:
                rsst['i'] = 1
            act(RSc(), MS[:, :], AF.Ln, r=['MS', 'SM'] + rkeys, w=[RSk()], scale=1.0 / nfeat, bias=SM[:, SM_EPS:SM_EPS + 1])
            act(RSc(), RSc(), AF.Exp, r=[RSk()], w=[RSk()], scale=-0.5)

        def prenorm_gen(gcol):
            for tc in range(NTC):
                prenorm(gcol, only_tc=tc, slot=tc)
                yield

        def prenorm(gcol, only_tc=None, slot=0):
            for tc in (range(NTC) if only_tc is None else [only_tc]):
                dtc = tc if only_tc is None else slot
                for c in range(8):
                    sq = B0 if (c % 2 == 0) else B1
                    sk_ = 'B0' if (c % 2 == 0) else 'B1'
                    act(sq[:, :], xs(c, tc), AF.Square, r=[xk(c, tc)], w=[sk_])
                    mm(MS[:, :], ones, sq[:, :], c == 0, c == 7, r=[sk_, 'CB'], w=['MS'])
                rstd_from_ms(1024.0, [])
                for c in range(8):
                    stt(hs(c, dtc * 512, 512), xs(c, tc), SM[:, gcol + c:gcol + c + 1], RSc(), ALU.mult, ALU.mult,
                        r=[xk(c, tc), RSk(), 'SM'], w=[hk(c, dtc)])

        def postnorm(tc, gcol):
            for c in range(8):
                sq = B0 if (c % 2 == 0) else B1
                sk_ = 'B0' if (c % 2 == 0) else 'B1'
                mx = mxap(c)
                act(sq[:, :], mx, AF.Square, r=mxkeys(c), w=[sk_])
                mm(MS[:, :], ones, sq[:, :], c == 0, c == 7, r=[sk_, 'CB'], w=['MS'])
            rstd_from_ms(1024.0, [])
            for c in range(8):
                mx = mxap(c)
                tt('pool', mx, mx, RSc(), ALU.mult, r=mxkeys(c) + [RSk()], w=mxkeys(c))
                stt(xs(c, tc), mx, SM[:, gcol + c:gcol + c + 1], xs(c, tc), ALU.mult, ALU.add,
                    r=mxkeys(c) + ['SM', xk(c, tc)], w=[xk(c, tc)])

        def qkbuf(b):
            QT = PH[:, (2 * b) * S:(2 * b + 1) * S]
            KT = PH[:, (2 * b + 1) * S:(2 * b + 2) * S]
            return QT, KT

        def attention_gen(b, R, h, dil, hcol, post=None):
            QT, KT = qkbuf(b)
            tiles = [(I, j) for I in range(4) for j in range(4 * I + 4)]
            info = {}

            def emit_qk(k):
                I, j = tiles[k]
                r0 = max(0, j - 4 * I)
                q0 = 512 * I + 128 * r0
                n = 512 - 128 * r0
                st = rr('st', 3)
                mm(STp[st][:, 0:n], KT[0:R, 128 * j:128 * j + 128], QT[0:R, q0:q0 + n], True, True,
                   r=[qkey(b, I), kkey(b, j // 4)], w=['ST%d' % st])
                info[k] = (st, r0, n)

            pinfo = {}

            def emit_exp(k):
                I, j = tiles[k]
                st, r0, n = info[k]
                ST = STp[st]
                stk = 'ST%d' % st
                pi = rr('pt', NPT)
                PTt = PT_[pi]
                ptk = 'PT%d' % pi
                act(PTt[:, 0:n], ST[:, 0:n], AF.Exp, r=[stk], w=[ptk])
                meng = 'pool' if (MASK_ALT and k % 2 == 1) else 'dve'
                if dil:
                    d0 = min(max(4 * I - j, 0), 5)
                    msk = CB[:, CB_DM + 128 * d0:CB_DM + 128 * d0 + n]
                    tt(meng, PTt[:, 0:n], PTt[:, 0:n], msk, ALU.mult, r=[ptk, 'CB'], w=[ptk])
                elif j >= 4 * I:
                    msk = CB[:, CB_CM:CB_CM + n]
                    tt(meng, PTt[:, 0:n], PTt[:, 0:n], msk, ALU.mult, r=[ptk, 'CB'], w=[ptk])
                pinfo[k] = (PTt, ptk)

            emit_qk(0)
            emit_qk(1)
            emit_exp(0)
            oa = 0
            for k, (I, j) in enumerate(tiles):
                if k + 2 < len(tiles):
                    emit_qk(k + 2)
                if k + 1 < len(tiles):
                    emit_exp(k + 1)
                if j == 0:
                    oa = rr('oa', 2)
                OA = OAp[oa]
                oak = 'OA%d' % oa
                st, r0, n = info[k]
                PTt, ptk = pinfo[k]
                for r_ in range(r0, 4):
                    i = 4 * I + r_
                    mm(OA[:, 65 * r_:65 * r_ + 65], PTt[:, 128 * (r_ - r0):128 * (r_ - r0) + 128],
                       Vv[:, j, h, :], (j == 0 and r_ == 0), j == i, r=[ptk] + vkeys(j), w=[oak], sgc=True)
                if j == 4 * I + 3:
                    OAv = OA[:, 0:260].rearrange("p (r d) -> p r d", r=4)
                    P.op('dve', lambda e, OAv=OAv: e.reciprocal(out=RINV, in_=OAv[:, :, 64]), r=[oak], w=['RINV'])
                    for r_ in range(4):
                        tb = 4 * I + r_
                        ts('dve', Ytok[:, tb * 128 + hcol:tb * 128 + hcol + 64], OA[:, 65 * r_:65 * r_ + 64],
                           RINV[:, r_:r_ + 1], ALU.mult, r=[oak, 'RINV'], w=['YK%d' % I])
                yield
            if post is not None:
                post()
                yield

        def run_jobs(jobs):
            pending = None
            for (pgi, pga, agf) in jobs:
                if pgi is not None:
                    g = pgi()
                    a_done = pending is None
                    b_done = False
                    while not (a_done and b_done):
                        if not a_done:
                            try:
                                next(pending)
                            except StopIteration:
                                a_done = True
                        if not b_done:
                            try:
                                next(g)
                            except StopIteration:
                                b_done = True
                elif pending is not None:
                    for _ in pending:
                        pass
                pending = None
                if pga is not None:
                    for _ in pga():
                        pass
                pending = agf()
            if pending is not None:
                for _ in pending:
                    pass

        def ytok_to_YT(chunk):
            for g4 in range(4):
                pj = rr('pj', 2)
                ps, pk = PJ[pj], 'PJ%d' % pj
                for k in range(4):
                    tb = 4 * g4 + k
                    mm(ps[:, 128 * k:128 * k + 128], Ytok[:, tb * 128:tb * 128 + 128], ident, True, True,
                       r=['YK%d' % g4, 'CB'], w=[pk])
                cp('act', ys(chunk, g4 * 512, 512), ps[:, :], r=[pk], w=[yk(4 * chunk + g4)])

        def v_project_gen(woff, wkey, ncols_tot):
            for tb2 in range(NTB // 2):
                pj = rr('pj', 2)
                ps = PJ[pj]
                pk = 'PJ%d' % pj
                for half in range(2):
                    tb = 2 * tb2 + half
                    for kc in range(8):
                        mm(ps[:, 256 * half:256 * half + 256], hs(kc, tb * 128, 128),
                           WR[:, woff + kc * ncols_tot:woff + kc * ncols_tot + 256], kc == 0, kc == 7,
                           r=[wkey, hk(kc, tb // 4)], w=[pk])
                cp('act', Vv[:, 2 * tb2:2 * tb2 + 2, :, 0:64],
                   ps[:, :].rearrange("p (t h d) -> p t h d", t=2, h=4), r=[pk], w=vkeys(2 * tb2) + vkeys(2 * tb2 + 1))
                yield

        hb = {'i': 0}

        def next_buf():
            b = hb['i'] % 2
            hb['i'] += 1
            return b

        def jobs_rope8(l, nm, n_mix, is_moba):
            jobs = []
            for h in range(4):
                b = next_buf()

                def pgi(h=h, b=b):
                    woff, wkey = w_get(l, '%s_h%d' % (nm, h))
                    w_prefetch()
                    QT, KT = qkbuf(b)
                    if is_moba:
                        P.op('pool', lambda e, QT=QT: e.memset(QT[96:120, 0:1024], 0.0),
                             w=[qkey(b, 0), qkey(b, 1)])
                        dma('pool', KT[112:120, :], indd[:, :], r=[], w=[kkey(b, t) for t in range(4)])
                    for which, tbl, dst, scale in (('k', CB_ROPE + 2048, KT, None), ('q', CB_ROPE, QT, 0.125)):
                        c0 = 0 if which == 'q' else 112
                        for tc in range(NTC):
                            pj = rr('pj', 2)
                            ps = PJ[pj]
                            pk = 'PJ%d' % pj
                            for kc in range(8):
                                mm(ps[0:112, :], WR[:, woff + kc * 224 + c0:woff + kc * 224 + c0 + 112], hs(kc, tc * 512, 512),
                                   kc == 0, kc == 7, r=[wkey, hk(kc, tc)], w=[pk])
                            dk = qkey(b, tc) if which == 'q' else kkey(b, tc)
                            tt('dve', dst[0:64, tc * 512:tc * 512 + 512], ps[0:64, :], CB[0:64, tbl + tc * 512:tbl + tc * 512 + 512],
                               ALU.mult, r=[pk, 'CB'], w=[dk])
                            if scale is None:
                                cp('dve' if EVAC_DVE else 'act', dst[64:112, tc * 512:tc * 512 + 512], ps[64:112, :], r=[pk], w=[dk])
                            else:
                                if EVAC_DVE:
                                    ts('dve', dst[64:112, tc * 512:tc * 512 + 512], ps[64:112, :], scale, ALU.mult, r=[pk], w=[dk])
                                else:
                                    act(dst[64:112, tc * 512:tc * 512 + 512], ps[64:112, :], AF.Copy, r=[pk], w=[dk], scale=scale)
                            yield
                    if is_moba:
                        moba_gate(b)
                        yield

                def pga():
                    woff, wkey = w_get(l, nm + '_v')
                    w_prefetch()
                    for _ in v_project_gen(woff, wkey, 256):
                        yield

                def agf(h=h, b=b):
                    post = (lambda: ytok_to_YT(2 * n_mix + h // 2)) if h % 2 == 1 else None
                    return attention_gen(b, 120 if is_moba else 112, h, nm == 'dil', (h % 2) * 64, post)

                jobs.append((pgi, pga if h == 0 else None, agf))
            return jobs

        def moba_gate(b):
            QT, KT = qkbuf(b)
            kk = [kkey(b, t) for t in range(4)]
            P.op('dve', lambda e: e.tensor_reduce(out=KSF[0:112, :], in_=KT[0:112, :].rearrange("p (n k) -> p n k", n=8),
                                                  axis=AX.X, op=ALU.add), r=kk, w=['KSF'])
            cp('dve', KSB[0:112, 0:8], KSF[0:112, :], r=['KSF'], w=['KSB'])
            tt('dve', KSB[0:112, 8:16], KSF[0:112, :], KSB[0:112, 0:8], ALU.subtract, r=['KSF', 'KSB'], w=['KSB'])
            for tbi in range(8):
                tb = 8 + tbi
                mm(MS[:, 8 * tbi:8 * tbi + 8], QT[0:112, tb * 128:tb * 128 + 128], KSB[0:112, 0:8], True, False,
                   r=['KSB', qkey(b, tb // 4)], w=['MS'])
                mm(MS[:, 8 * tbi:8 * tbi + 8], QT[0:112, tb * 128:tb * 128 + 128], KSB[0:112, 8:16], False, True,
                   r=['KSB', qkey(b, tb // 4)], w=['MS'])
            tt('dve', GT, MS[:, 0:64], SM[:, SM_NEG:SM_NEG + 64], ALU.add, r=['MS', 'SM'], w=['GT'])
            for tbi in range(8):
                P.op('dve', lambda e, tbi=tbi: e.max(out=TOP8[:, 8 * tbi:8 * tbi + 8], in_=GT[:, 8 * tbi:8 * tbi + 8]),
                     r=['GT'], w=['TOP8'])
            for tbi in range(8):
                ts('dve', PEN[:, 8 * tbi:8 * tbi + 8], GT[:, 8 * tbi:8 * tbi + 8], TOP8[:, 8 * tbi + 2:8 * tbi + 3], ALU.is_lt,
                   r=['GT', 'TOP8'], w=['PEN'], s2=-30000.0, op1=ALU.mult)
            tt('dve', PEN[:, :], PEN[:, :], SM[:, SM_VAL:SM_VAL + 64], ALU.mult, r=['PEN', 'SM'], w=['PEN'])
            for half in range(2):
                for k in range(4):
                    tbi = 4 * half + k
                    mm(MS[0:8, 128 * k:128 * k + 128], PEN[:, 8 * tbi:8 * tbi + 8], ident, True, True,
                       r=['PEN', 'CB'], w=['MS'])
                cp('dve', PENT[:, :], MS[0:8, :], r=['MS'], w=['PENT'])
                dma('sp', QT[112:120, 1024 + 512 * half:1024 + 512 * half + 512], PENT[:, :], r=['PENT'],
                    w=[qkey(b, 2 + half)])

        def jobs_fox(l):
            jobs = []
            wst = {}
            for h in range(4):
                b = next_buf()

                def pgi(h=h, b=b):
                    if h == 0:
                        wof, wkf = w_get(l, 'fox_f')
                        w_prefetch()
                        P.op('dve', lambda e: e.tensor_copy(out=FWT[:, :], in_=WR[:, wof:wof + 128]), r=[wkf], w=['FWT'])
                    woff, wkey = w_get(l, 'fox_h%d' % h)
                    w_prefetch()
                    QT, KT = qkbuf(b)
                    hoff = woff
                    prev_cs = None
                    for tc in range(NTC):
                        pj = rr('pj', 2)
                        ps = PJ[pj]
                        pk = 'PJ%d' % pj
                        for kc in range(8):
                            mm(ps[:, :], WR[:, hoff + kc * 128:hoff + kc * 128 + 128], hs(kc, tc * 512, 512), kc == 0, kc == 7,
                               r=[wkey, hk(kc, tc)], w=[pk])
                        if EVAC2_DVE:
                            ts('dve', QT[0:64, tc * 512:tc * 512 + 512], ps[0:64, :], 0.125, ALU.mult, r=[pk], w=[qkey(b, tc)])
                        else:
                            act(QT[0:64, tc * 512:tc * 512 + 512], ps[0:64, :], AF.Copy, r=[pk], w=[qkey(b, tc)], scale=0.125)
                        cp('dve', KT[0:64, tc * 512:tc * 512 + 512], ps[64:128, :], r=[pk], w=[kkey(b, tc)])
                        yield
                        pj = rr('pj', 2)
                        ps = PJ[pj]
                        pk = 'PJ%d' % pj
                        for kc in range(8):
                            mm(ps[0:4, :], FWT[:, kc * 16 + 4 * h:kc * 16 + 4 * h + 4], hs(kc, tc * 512, 512), kc == 0, kc == 7,
                               r=['FWT', hk(kc, tc)], w=[pk])
                        U_ = T0[0:4, :]
                        E_ = T1[0:4, :]
                        csb = FX[0:4, 512 * (tc % 2):512 * (tc % 2) + 512]
                        csk = 'FXc%d' % (tc % 2)
                        TMP = FXB[0:4, :]
                        ts('dve', U_, ps[0:4, :], SM[0:4, SM_BF + 4 * l + h:SM_BF + 4 * l + h + 1], ALU.add, r=[pk, 'SM'], w=['T0'])
                        act(E_, U_, AF.Exp, r=['T0'], w=['T1'], scale=-1.0)
                        act(U_, E_, AF.Ln, r=['T1', 'SM'], w=['T0'], scale=1.0, bias=SM[0:4, SM_ONE:SM_ONE + 1])
                        init = 0.0 if tc == 0 else prev_cs
                        P.op('dve', lambda e, csb=csb, U_=U_, init=init: e.tensor_tensor_scan(out=csb, data0=ONES4, data1=U_,
                                                                                             initial=init, op0=ALU.mult, op1=ALU.add),
                             r=['T0', 'CB'] + ([] if tc == 0 else ['FXc%d' % ((tc - 1) % 2)]), w=[csk])
                        prev_cs = csb[:, 511:512]
                        HI = B0[0:4, :]
                        LO = B1[0:4, :]
                        cp('dve', HI, csb, r=[csk], w=['B0'])
                        tt('dve', LO, csb, HI, ALU.subtract, r=[csk, 'B0'], w=['B1'])
                        A = SM_AUG
                        ts('dve', TMP, HI, SM[0:4, A:A + 1], ALU.mult, r=['B0', 'SM'], w=['FXt'], s2=SM[0:4, A + 2:A + 3], op1=ALU.add)
                        stt(QT[64:68, tc * 512:tc * 512 + 512], LO, SM[0:4, A + 1:A + 2], TMP, ALU.mult, ALU.add,
                            r=['B1', 'FXt', 'SM'], w=[qkey(b, tc)])
                        ts('dve', TMP, HI, SM[0:4, A + 3:A + 4], ALU.mult, r=['B0', 'SM'], w=['FXt'], s2=SM[0:4, A + 5:A + 6], op1=ALU.add)
                        stt(KT[64:68, tc * 512:tc * 512 + 512], LO, SM[0:4, A + 4:A + 5], TMP, ALU.mult, ALU.add,
                            r=['B1', 'FXt', 'SM'], w=[kkey(b, tc)])
                        yield

                def pga():
                    woff, wkey = w_get(l, 'fox_v')
                    w_prefetch()
                    for _ in v_project_gen(woff, wkey, 256):
                        yield

                def agf(h=h, b=b):
                    post = (lambda: ytok_to_YT(0 + h // 2)) if h % 2 == 1 else None
                    return attention_gen(b, 68, h, False, (h % 2) * 64, post)

                jobs.append((pgi, pga if h == 0 else None, agf))
            return jobs

        def jobs_mla(l, pn=None):
            CQ0, CKV = 0, 4
            scm = 96.0 ** -0.5
            wst = {}

            def pre():
                woff, wkey = w_get(l, 'mla_lat')
                w_prefetch()
                for tc in range(NTC):
                    if pn is not None:
                        next(pn)
                    pss = []
                    for cc in range(2):
                        pj = rr('pj', 2)
                        ps = PJ[pj]
                        pk = 'PJ%d' % pj
                        pss.append((ps, pk))
                        for kc in range(8):
                            mm(ps[:, :], WR[:, woff + kc * 384 + cc * 128:woff + kc * 384 + cc * 128 + 128], hs(kc, tc * 512, 512),
                               kc == 0, kc == 7, r=[wkey, hk(kc, tc)], w=[pk])
                        sq = B0 if cc == 0 else B1
                        sk_ = 'B0' if cc == 0 else 'B1'
                        act(sq[:, :], ps[:, :], AF.Square, r=[pk], w=[sk_])
                        mm(MS[:, :], ones, sq[:, :], cc == 0, cc == 1, r=[sk_, 'CB'], w=['MS'])
                    rstd_from_ms(256.0, [])
                    for cc in range(2):
                        ps, pk = pss[cc]
                        stt(ys(CQ0 + cc, tc * 512, 512), ps[:, :], SM[:, SM_GCQ + 2 * l + cc:SM_GCQ + 2 * l + cc + 1], RSc(),
                            ALU.mult, ALU.mult, r=[pk, RSk(), 'SM'], w=[yk(4 * (CQ0 + cc) + tc)])
                    pj = rr('pj', 2)
                    ps = PJ[pj]
                    pk = 'PJ%d' % pj
                    for kc in range(8):
                        mm(ps[:, :], WR[:, woff + kc * 384 + 256:woff + kc * 384 + 384], hs(kc, tc * 512, 512), kc == 0, kc == 7,
                           r=[wkey, hk(kc, tc)], w=[pk])
                    act(B0[:, :], ps[:, :], AF.Square, r=[pk], w=['B0'])
                    mm(MS[:, :], ones, B0[:, :], True, True, r=['B0', 'CB'], w=['MS'])
                    rstd_from_ms(128.0, [])
                    stt(ys(CKV, tc * 512, 512), ps[:, :], SM[:, SM_GCKV + l:SM_GCKV + l + 1], RSc(), ALU.mult, ALU.mult,
                        r=[pk, RSk(), 'SM'], w=[yk(4 * CKV + tc)])
                    yield
                woff, wkey = w_get(l, 'mla_kr')
                w_prefetch()
                for tc in range(NTC):
                    pj1 = rr('pj', 2)
                    p1, k1 = PJ[pj1], 'PJ%d' % pj1
                    for kc in range(8):
                        mm(p1[:, :], WR[:, woff + kc * 256:woff + kc * 256 + 128], hs(kc, tc * 512, 512), kc == 0, kc == 7,
                           r=[wkey, hk(kc, tc)], w=[k1])
                    pj2 = rr('pj', 2)
                    p2, k2 = PJ[pj2], 'PJ%d' % pj2
                    for kc in range(8):
                        mm(p2[:, :], WR[:, woff + kc * 256 + 128:woff + kc * 256 + 256], hs(kc, tc * 512, 512), kc == 0, kc == 7,
                           r=[wkey, hk(kc, tc)], w=[k2])
                    ta = CB[64:128, CB_ROPE + 2048 + tc * 512:CB_ROPE + 2048 + tc * 512 + 512]
                    tb_ = CB[64:128, CB_ROPE + 4096 + tc * 512:CB_ROPE + 4096 + tc * 512 + 512]
                    tt('dve', T0[64:128, :], p2[64:128, :], tb_, ALU.mult, r=[k2, 'CB'], w=['T0'])
                    tt('dve', T1[64:128, :], p1[64:128, :], ta, ALU.mult, r=[k1, 'CB'], w=['T1'])
                    tt('dve', CB[0:64, CB_ROPE + 4096 + tc * 512:CB_ROPE + 4096 + tc * 512 + 512], T0[64:128, :], T1[64:128, :],
                       ALU.add, r=['T0', 'T1'], w=['KR%d' % tc])
                    yield
                wst['w'] = w_get(l, 'mla_up')
                w_prefetch()
                woff, wkey = wst['w']
                for tb2 in range(NTB // 2):
                    pj = rr('pj', 2)
                    ps = PJ[pj]
                    pk = 'PJ%d' % pj
                    for half in range(2):
                        tb = 2 * tb2 + half
                        mm(ps[:, 256 * half:256 * half + 256], ys(CKV, tb * 128, 128), WR[:, woff + 256:woff + 512], True, True,
                           r=[wkey, yk(4 * CKV + tb // 4)], w=[pk])
                    cp('act', Vv[:, 2 * tb2:2 * tb2 + 2, :, 0:64], ps[:, :].rearrange("p (t h d) -> p t h d", t=2, h=4),
                       r=[pk], w=vkeys(2 * tb2) + vkeys(2 * tb2 + 1))
                    yield

            jobs = []
            for h in range(4):
                b = next_buf()

                def pgi(h=h, b=b):
                    woff, wkey = wst['w']
                    QT, KT = qkbuf(b)
                    for tc in range(NTC):
                        pj = rr('pj', 2)
                        ps, pk = PJ[pj], 'PJ%d' % pj
                        mm(ps[0:64, :], WR[:, woff + 64 * h:woff + 64 * h + 64], ys(CKV, tc * 512, 512), True, True,
                           r=[wkey, yk(4 * CKV + tc)], w=[pk])
                        cp('dve' if EVAC2_DVE else 'act', KT[0:64, tc * 512:tc * 512 + 512], ps[0:64, :], r=[pk], w=[kkey(b, tc)])
                        cp('dve', KT[64:128, tc * 512:tc * 512 + 512],
                           CB[0:64, CB_ROPE + 4096 + tc * 512:CB_ROPE + 4096 + tc * 512 + 512], r=['KR%d' % tc], w=[kkey(b, tc)])
                        pj = rr('pj', 2)
                        ps, pk = PJ[pj], 'PJ%d' % pj
                        for kc in range(2):
                            mm(ps[:, :], WR[:, woff + 512 + kc * 512 + 128 * h:woff + 512 + kc * 512 + 128 * h + 128],
                               ys(CQ0 + kc, tc * 512, 512), kc == 0, kc == 1, r=[wkey, yk(4 * (CQ0 + kc) + tc)], w=[pk])
                        if EVAC2_DVE:
                            ts('dve', QT[0:64, tc * 512:tc * 512 + 512], ps[0:64, :], scm, ALU.mult, r=[pk], w=[qkey(b, tc)])
                        else:
                            act(QT[0:64, tc * 512:tc * 512 + 512], ps[0:64, :], AF.Copy, r=[pk], w=[qkey(b, tc)], scale=scm)
                        tt('dve', QT[64:128, tc * 512:tc * 512 + 512], ps[64:128, :],
                           CB[64:128, CB_ROPE + tc * 512:CB_ROPE + tc * 512 + 512], ALU.mult, r=[pk, 'CB'], w=[qkey(b, tc)])
                        yield

                def agf(h=h, b=b):
                    post = (lambda: ytok_to_YT(6 + h // 2)) if h % 2 == 1 else None
                    return attention_gen(b, 128, h, False, (h % 2) * 64, post)

                if h == 0:
                    def pga0(pgi=pgi):
                        for _ in pre():
                            yield
                        for _ in pgi():
                            yield
                    jobs.append((None, pga0, agf))
                else:
                    jobs.append((pgi, None, agf))
            return jobs

        def merge(l, hf):
            for d in range(8):
                for npair in range(2):
                    woff, wkey = w_get(l, 'mg_%d_%d' % (d, npair))
                    w_prefetch()
                    for nn in range(2):
                        n = 2 * npair + nn
                        go = woff + nn * 1280
                        bo = go + 1024
                        for t2 in range(2):
                            tc = 2 * hf + t2
                            pj = rr('pj', 2)
                            pg, kg = PJ[pj], 'PJ%d' % pj
                            for kc in range(8):
                                mm(pg[:, :], WR[:, go + kc * 128:go + kc * 128 + 128], hs(kc, tc * 512, 512), kc == 0, kc == 7,
                                   r=[wkey, hk(kc, tc)], w=[kg])
                            st = rr('st', 3)
                            pu, ku = STp[st], 'ST%d' % st
                            for kc in range(2):
                                mm(pu[:, :], WR[:, bo + kc * 128:bo + kc * 128 + 128], ys(2 * n + kc, tc * 512, 512), kc == 0, kc == 1,
                                   r=[wkey, yk(4 * (2 * n + kc) + tc)], w=[ku])
                            sg, sgk = (T0, 'T0') if (t2 == 0) else (T1, 'T1')
                            act(sg[:, :], pg[:, :], AF.Sigmoid, r=[kg], w=[sgk])
                            acc = accap(t2)
                            acck = acckeys(t2)
                            if n == 0:
                                tt('dve', acc, pu[:, :], sg[:, :], ALU.mult, r=[ku, sgk], w=acck)
                            else:
                                tt('dve', sg[:, :], pu[:, :], sg[:, :], ALU.mult, r=[ku, sgk], w=[sgk])
                                tt('pool', acc, acc, sg[:, :], ALU.add, r=acck + [sgk], w=acck)
                            if n == 3:
                                cp('act', mgap(hf, d, t2), acc, r=acck, w=[mgkey(hf, d, t2)])

        def wout(l):
            wo = []
            for u in range(2):
                wo.append(w_get(l, 'wo_%d' % u))
            for hf in range(2):
                for t2 in range(2):
                    tc = 2 * hf + t2
                    for dc in range(8):
                        woff, wkey = wo[dc // 4]
                        pj = rr('pj', 2)
                        ps, pk = PJ[pj], 'PJ%d' % pj
                        for kc in range(8):
                            mm(ps[:, :], WR[:, woff + kc * 512 + (dc % 4) * 128:woff + kc * 512 + (dc % 4) * 128 + 128],
                               mgap(hf, kc, t2), kc == 0, kc == 7, r=[wkey, mgkey(hf, kc, t2)], w=[pk])
                        cp('act', mxap(dc), ps[:, :], r=[pk], w=mxkeys(dc))
                    postnorm(tc, SM_GQM + 8 * l)
            w_prefetch()

        def mlp(l):
            gpre = SM_GPL + 8 * l
            gpost = SM_GQL + 8 * l

            def h2ap(kc, sub):
                return PH[:, kc * 1024 + sub * 512:kc * 1024 + sub * 512 + 512]

            def h2key(kc, sub):
                return 'PH%d' % (2 * kc + sub)

            def mx2(dc, sub):
                return hT[:, dc * S + sub * 1024:dc * S + sub * 1024 + 1024].bitcast(F32)

            def mx2keys(dc, sub):
                return [hk(dc, 2 * sub), hk(dc, 2 * sub + 1)]

            for th in range(2):
                for sub in range(2):
                    tc = 2 * th + sub
                    for c in range(8):
                        sq = B0 if (c % 2 == 0) else B1
                        sk_ = 'B0' if (c % 2 == 0) else 'B1'
                        act(sq[:, :], xs(c, tc), AF.Square, r=[xk(c, tc)], w=[sk_])
                        mm(MS[:, :], ones, sq[:, :], c == 0, c == 7, r=[sk_, 'CB'], w=['MS'])
                    rstd_from_ms(1024.0, [])
                    for c in range(8):
                        stt(h2ap(c, sub), xs(c, tc), SM[:, gpre + c:gpre + c + 1], RSc(), ALU.mult, ALU.mult,
                            r=[xk(c, tc), RSk(), 'SM'], w=[h2key(c, sub)])
                for fh in range(2):
                    for u in range(4 * fh, 4 * fh + 4):
                        woff, wkey = w_get(l, 'up_%d' % u)
                        w_prefetch()
                        for fq in range(4):
                            fcl = 4 * (u - 4 * fh) + fq
                            for sub in range(2):
                                pj = rr('pj', 2)
                                ps, pk = PJ[pj], 'PJ%d' % pj
                                for kc in range(8):
                                    mm(ps[:, :], WR[:, woff + kc * 512 + fq * 128:woff + kc * 512 + fq * 128 + 128], h2ap(kc, sub),
                                       kc == 0, kc == 7, r=[wkey, h2key(kc, sub)], w=[pk])
                                rl, rk = (B0, 'B0') if (sub == 0) else (B1, 'B1')
                                act(rl[:, :], ps[:, :], AF.Relu, r=[pk], w=[rk])
                                blk = 2 * fcl + sub
                                tt(SQENG, YT[:, blk * 512:blk * 512 + 512], rl[:, :], rl[:, :], ALU.mult, r=[rk], w=[yk(blk)])
                    for q in range(4):
                        woff, wkey = w_get(l, 'dn_%d_%d' % (fh, q))
                        w_prefetch()
                        for dq in range(2):
                            dc = 2 * q + dq
                            for sub in range(2):
                                pj = rr('pj', 2)
                                ps, pk = PJ[pj], 'PJ%d' % pj
                                for fcl in range(16):
                                    blk = 2 * fcl + sub
                                    mm(ps[:, :], WR[:, woff + fcl * 256 + dq * 128:woff + fcl * 256 + dq * 128 + 128],
                                       YT[:, blk * 512:blk * 512 + 512], fcl == 0, fcl == 15, r=[wkey, yk(blk)], w=[pk])
                                if fh == 0:
                                    cp('act', mx2(dc, sub), ps[:, :], r=[pk], w=mx2keys(dc, sub))
                                else:
                                    tt('dve', mx2(dc, sub), ps[:, :], mx2(dc, sub), ALU.add, r=[pk] + mx2keys(dc, sub),
                                       w=mx2keys(dc, sub))
                for sub in range(2):
                    tc = 2 * th + sub
                    for c in range(8):
                        sq = B0 if (c % 2 == 0) else B1
                        sk_ = 'B0' if (c % 2 == 0) else 'B1'
                        mx = mx2(c, sub)
                        act(sq[:, :], mx, AF.Square, r=mx2keys(c, sub), w=[sk_])
                        mm(MS[:, :], ones, sq[:, :], c == 0, c == 7, r=[sk_, 'CB'], w=['MS'])
                    rstd_from_ms(1024.0, [])
                    for c in range(8):
                        mx = mx2(c, sub)
                        tt('pool', mx, mx, RSc(), ALU.mult, r=mx2keys(c, sub) + [RSk()], w=mx2keys(c, sub))
                        stt(xs(c, tc), mx, SM[:, gpost + c:gpost + c + 1], xs(c, tc), ALU.mult, ALU.add,
                            r=mx2keys(c, sub) + ['SM', xk(c, tc)], w=[xk(c, tc)])

        for s_ in range(nseq):
            for c in range(8):
                dma('sp', xT[:, c * S:(c + 1) * S], xin[s_, :, c * S:(c + 1) * S], r=[], w=[xk(c, t) for t in range(4)])
            for l in range(nlayers):
                pn = None
                if 'p' in phases:
                    if PN_INTER and 'a' in phases:
                        pn = prenorm_gen(SM_GPM + 8 * l)
                    else:
                        prenorm(SM_GPM + 8 * l)
                v_set_ones()
                jobs = []
                if 'a' in phases:
                    jobs += jobs_mla(l, pn)
                if 'd' in phases:
                    jobs += jobs_rope8(l, 'dil', 1, False)
                if 'm' in phases:
                    jobs += jobs_rope8(l, 'mob', 2, True)
                if 'f' in phases:
                    jobs += jobs_fox(l)
                run_jobs(jobs)
                if debug and s_ == 0 and l == 0:
                    for c in range(8):
                        if phases == 'p':
                            dma('pool', dbg[:, c * S:(c + 1) * S], hT[:, c * S:(c + 1) * S], r=[hk(c, t) for t in range(4)], w=[])
                        else:
                            dma('pool', dbg[:, c * S:(c + 1) * S], YT[:, c * S:(c + 1) * S], r=[yk(4 * c + t) for t in range(4)], w=[])
                if 'g' in phases:
                    merge(l, 0)
                    merge(l, 1)
                if 'o' in phases:
                    wout(l)
                if 'l' in phases:
                    mlp(l)
            for c in range(8):
                dma('sp', yout[s_, :, c * S:(c + 1) * S], xT[:, c * S:(c + 1) * S], r=[xk(c, t) for t in range(4)], w=[])
        P.emit()
    return nc


def _to_fm(xs):
    n = xs.shape[0]
    return np.ascontiguousarray(xs.transpose(0, 2, 1).reshape(n, 8, 128, S).transpose(0, 2, 1, 3)).reshape(n, 128, 8 * S)


def _from_fm(yT):
    n = yT.shape[0]
    return yT.reshape(n, 128, 8, S).transpose(0, 2, 1, 3).reshape(n, D, S).transpose(0, 2, 1)


def kernel(**inputs):
    inp = {k: np.asarray(v) for k, v in inputs.items()}
    x = inp['x'].astype(np.float32, copy=False)
    nseq = x.shape[0] // NCORE
    wts = build_weights(inp, DEPTH)
    sm = build_small(inp)
    cb, ind = build_consts()
    nc = build_program(nseq, DEPTH)
    in_maps = []
    for c in range(NCORE):
        in_maps.append({'xT': _to_fm(x[c * nseq:(c + 1) * nseq]), 'wts': wts, 'small': sm, 'cb': cb, 'ind': ind})
    res = run_bass_kernel_spmd(nc, in_maps, core_ids=list(range(NCORE)))
    out = np.empty_like(x)
    for c in range(NCORE):
        out[c * nseq:(c + 1) * nseq] = _from_fm(np.asarray(res.results[c]['yT']))
    return out
```

```python
import contextlib
import numpy as np
import concourse.bass as bass
import concourse.mybir as mybir
from concourse.bass_utils import run_bass_kernel_spmd

F32 = mybir.dt.float32
BF16 = mybir.dt.bfloat16
AF = mybir.ActivationFunctionType
ALU = mybir.AluOpType
AX = mybir.AxisListType

S = 2048
D = 1024
NCORE = 8
DEPTH = 4
NTC = 4
NTB = 16
WSLOT = 4096
NSLOT = 2

C_FQ, C_FK, C_FV, C_FF = 0, 256, 512, 768
C_DQ, C_DK, C_DV = 772, 1028, 1284
C_MQ, C_MK, C_MV = 1540, 1796, 2052
C_CQ, C_CKV, C_KR, C_G = 2308, 2564, 2692, 2724

COMPUTE = ('pe', 'act', 'dve', 'pool')
NDS = 12


class Op:
    __slots__ = ('eng', 'fn', 'deps', 'dma', 'idx', 'signal', 'count', 'waits', 'dma_id')

    def __init__(self, eng, fn, dma):
        self.eng = eng
        self.fn = fn
        self.dma = dma
        self.deps = []
        self.signal = False
        self.count = 0
        self.waits = []
        self.dma_id = -1


class Prog:
    def __init__(self, nc):
        self.nc = nc
        self.ops = []
        self.last_w = {}
        self.readers = {}
        self.n_dma = [0, 0]

    def op(self, eng, fn, r=(), w=(), dma=False):
        o = Op(eng, fn, dma)
        o.idx = len(self.ops)
        deps = {}
        for k in r:
            p = self.last_w.get(k)
            if p is not None:
                deps[p] = 'raw'
        for k in w:
            p = self.last_w.get(k)
            if p is not None and p not in deps:
                deps[p] = 'waw'
            for rd in self.readers.get(k, ()):
                if rd not in deps:
                    deps[rd] = 'war'
        if dma:
            cls = 1 if eng == 'pool' else 0
            o.dma_id = (cls, self.n_dma[cls])
            self.n_dma[cls] += 1
        o.deps = list(deps.items())
        for k in w:
            self.last_w[k] = o.idx
            self.readers[k] = []
        for k in r:
            lst = self.readers.setdefault(k, [])
            if not dma:
                lst[:] = [x for x in lst if self.ops[x].dma or self.ops[x].eng != eng]
            lst.append(o.idx)
        self.ops.append(o)
        return o

    @staticmethod
    def _needs_wait(cons, prod, kind):
        if prod.dma or cons.dma:
            return True
        if prod.eng != cons.eng:
            return True
        if cons.eng == 'pe':
            return False
        return True

    def finalize(self):
        ops = self.ops
        for o in ops:
            for (p, kind) in o.deps:
                po = ops[p]
                if self._needs_wait(o, po, kind) and not po.dma:
                    po.signal = True
        cnt = {e: 0 for e in COMPUTE}
        for o in ops:
            if (not o.dma) and o.signal:
                cnt[o.eng] += 1
                o.count = cnt[o.eng]
        waited = {}
        waited_dma = {}
        for o in ops:
            need = {}
            need_dma = {}
            for (p, kind) in o.deps:
                po = ops[p]
                if not self._needs_wait(o, po, kind):
                    continue
                if po.dma:
                    need_dma[po.dma_id] = True
                else:
                    need[po.eng] = max(need.get(po.eng, 0), po.count)
            if o.dma and o.dma_id[1] >= NDS:
                need_dma[(o.dma_id[0], o.dma_id[1] - NDS)] = True
            w = []
            for e, c in need.items():
                if waited.get((o.eng, e), 0) < c:
                    waited[(o.eng, e)] = c
                    w.append(('c', e, c))
            for d in need_dma:
                s = d[0] * NDS + d[1] % NDS
                tgt = 16 * (d[1] // NDS + 1)
                if waited_dma.get((o.eng, s), 0) < tgt:
                    waited_dma[(o.eng, s)] = tgt
                    w.append(('d', s, tgt))
            o.waits = w
        self.final_counts = cnt

    def emit(self):
        nc = self.nc
        self.finalize()
        with contextlib.ExitStack() as es:
            csem = {e: es.enter_context(nc.semaphore('cs_' + e)) for e in COMPUTE}
            dsem = [es.enter_context(nc.semaphore('ds_%d' % i)) for i in range(2 * NDS)]
            block = es.enter_context(nc.Block())
            by_eng = {}
            for o in self.ops:
                by_eng.setdefault(o.eng, []).append(o)
            ndma = self.n_dma

            def run(ename, e):
                for o in by_eng.get(ename, []):
                    for (t, a, b) in o.waits:
                        if t == 'c':
                            e.wait_ge(csem[a], b)
                        else:
                            e.wait_ge(dsem[a], b)
                    ins = o.fn(e)
                    if o.dma:
                        ins.then_inc(dsem[o.dma_id[0] * NDS + o.dma_id[1] % NDS], 16)
                    elif o.signal:
                        ins.then_inc(csem[o.eng], 1)
                if ename == 'sp':
                    for cls in range(2):
                        for s in range(NDS):
                            n = (ndma[cls] - s + NDS - 1) // NDS if ndma[cls] > s else 0
                            if n > 0:
                                e.wait_ge(dsem[cls * NDS + s], 16 * n)
                    for ce in COMPUTE:
                        if self.final_counts[ce] > 0:
                            e.wait_ge(csem[ce], self.final_counts[ce])

            @block.tensor
            def _(e):
                run('pe', e)

            @block.scalar
            def _(e):
                run('act', e)

            @block.vector
            def _(e):
                run('dve', e)

            @block.gpsimd
            def _(e):
                run('pool', e)

            @block.sync
            def _(e):
                run('sp', e)


def _st(w2d, cols):
    K = w2d.shape[0]
    nk = K // 128
    cols = np.asarray(cols, dtype=np.int64)
    wp = np.concatenate([w2d, np.zeros((K, 1), w2d.dtype)], axis=1)
    g = wp[:, cols]
    M = len(cols)
    return np.ascontiguousarray(g.reshape(nk, 128, M).transpose(1, 0, 2)).reshape(128, nk * M)


def _rope8_cols(base, h, kind):
    if kind == 'q':
        feat = [0, 0, 1, 1, 0, 0, 1, 1]
    else:
        feat = [0, 0, 1, 1, 1, 1, 0, 0]
    cols = []
    for e in range(8):
        for f in range(8):
            cols.append(base + h * 64 + feat[e] * 8 + f)
    for d in range(16, 64):
        cols.append(base + h * 64 + d)
    return cols


def unit_defs():
    U = {}

    U['mla_lat'] = (8 * 384, lambda W: _st(W['w_in'], list(range(C_CQ, C_CQ + 256)) + list(range(C_CKV, C_CKV + 128))))

    def kr_cols():
        p1, p2 = [-1] * 64, [-1] * 64
        f1 = [0, 1, 1, 0]
        f2 = [1, 0, 0, 1]
        for e in range(4):
            for f in range(16):
                p1.append(C_KR + f1[e] * 16 + f)
        for e in range(4):
            for f in range(16):
                p2.append(C_KR + f2[e] * 16 + f)
        return p1 + p2
    U['mla_kr'] = (8 * 256, lambda W: _st(W['w_in'], kr_cols()))

    def mla_up(W):
        a = _st(W['w_uk'], list(range(256)))
        b = _st(W['w_uv'], list(range(256)))
        cols = []
        fq = [0, 0, 1, 1]
        for h in range(4):
            for d in range(64):
                cols.append(h * 96 + d)
            for e in range(4):
                for f in range(16):
                    cols.append(h * 96 + 64 + fq[e] * 16 + f)
        c = _st(W['w_uq'], cols)
        return np.concatenate([a, b, c], axis=1)
    U['mla_up'] = (512 + 1024, mla_up)

    for nm, cq, ck, cv in (('dil', C_DQ, C_DK, C_DV), ('mob', C_MQ, C_MK, C_MV)):
        U[nm + '_v'] = (8 * 256, (lambda W, cv=cv: _st(W['w_in'], list(range(cv, cv + 256)))))
        for h in range(4):
            U['%s_h%d' % (nm, h)] = (8 * 224, (lambda W, cq=cq, ck=ck, h=h:
                                                _st(W['w_in'], _rope8_cols(cq, h, 'q') + _rope8_cols(ck, h, 'k'))))

    def fox_f(W):
        cols = []
        for h in range(4):
            cols += [C_FF + h] * 4
        return _st(W['w_in'], cols)
    U['fox_f'] = (8 * 16, fox_f)
    U['fox_v'] = (8 * 256, lambda W: _st(W['w_in'], list(range(C_FV, C_FV + 256))))
    for h in range(4):
        U['fox_h%d' % h] = (8 * 128, (lambda W, h=h: _st(W['w_in'], list(range(C_FQ + h * 64, C_FQ + h * 64 + 64)) +
                                                        list(range(C_FK + h * 64, C_FK + h * 64 + 64)))))

    for d in range(8):
        for npair in range(2):
            def mg(W, d=d, npair=npair):
                parts = []
                for n in (2 * npair, 2 * npair + 1):
                    parts.append(_st(W['w_in'], list(range(C_G + n * 1024 + d * 128, C_G + n * 1024 + d * 128 + 128))))
                    parts.append(_st(W['w_branch'][n], list(range(d * 128, d * 128 + 128))))
                return np.concatenate(parts, axis=1)
            U['mg_%d_%d' % (d, npair)] = (2 * 1280, mg)
    for u in range(2):
        U['wo_%d' % u] = (8 * 512, (lambda W, u=u: _st(W['w_out'], list(range(u * 512, u * 512 + 512)))))
    for u in range(8):
        U['up_%d' % u] = (8 * 512, (lambda W, u=u: _st(W['w_up'], list(range(u * 512, u * 512 + 512)))))
    for fh in range(2):
        for q in range(4):
            U['dn_%d_%d' % (fh, q)] = (16 * 256, (lambda W, fh=fh, q=q: _st(W['w_down'][fh * 2048:(fh + 1) * 2048],
                                                                          list(range(q * 256, q * 256 + 256)))))
    return U


def unit_order():
    o = ['mla_lat', 'mla_kr', 'mla_up']
    for nm in ('dil', 'mob'):
        o += [nm + '_h0', nm + '_v', nm + '_h1', nm + '_h2', nm + '_h3']
    o += ['fox_f', 'fox_h0', 'fox_v', 'fox_h1', 'fox_h2', 'fox_h3']
    for hf in range(2):
        for d in range(8):
            for npair in range(2):
                o.append('mg_%d_%d' % (d, npair))
    o += ['wo_0', 'wo_1']
    for th in range(2):
        for fh in range(2):
            o += ['up_%d' % u for u in range(4 * fh, 4 * fh + 4)]
            o += ['dn_%d_%d' % (fh, q) for q in range(4)]
    return o


UNITS = unit_defs()
UOFF = {}
_off = 0
for _k, (_f, _) in UNITS.items():
    UOFF[_k] = _off
    _off += _f
FTOT = _off

SM_GPM, SM_GQM, SM_GPL, SM_GQL = 0, 32, 64, 96
SM_GCQ, SM_GCKV, SM_BF, SM_AUG, SM_EPS, SM_NEG, SM_VAL, SM_ONE = 128, 136, 140, 156, 162, 163, 227, 291
NSM = 292

CB_ROPE = 0
CB_DM = 3 * 2048
CB_CM = CB_DM + 9 * 128
CB_ID = CB_CM + 512
CB_ONE = CB_ID + 128
NCB = CB_ONE + 512


def build_consts():
    cb = np.zeros((128, NCB), np.float32)
    t = np.arange(S, dtype=np.float64)
    invf = 1.0 / (500000.0 ** (np.arange(0, 16, 2, dtype=np.float64) / 16.0))
    ang = np.float32(t)[None, :].astype(np.float64) * np.float32(invf)[:, None].astype(np.float64)
    c, s = np.cos(ang), np.sin(ang)
    tq = [c, s, c, s, s, c, s, c]
    tk = [c, s, c, s, c, -s, -c, s]
    A = cb[:, CB_ROPE:CB_ROPE + 2048]
    B = cb[:, CB_ROPE + 2048:CB_ROPE + 4096]
    Cc = cb[:, CB_ROPE + 4096:CB_ROPE + 6144]
    for e in range(8):
        A[8 * e:8 * e + 8] = 0.125 * tq[e]
        B[8 * e:8 * e + 8] = tk[e]
    invm = 1.0 / (500000.0 ** (np.arange(0, 32, 2, dtype=np.float64) / 32.0))
    angm = np.float32(t)[None, :].astype(np.float64) * np.float32(invm)[:, None].astype(np.float64)
    cm, sm = np.cos(angm), np.sin(angm)
    sc = 96.0 ** -0.5
    tq4 = [cm, sm, cm, sm]
    ta = [cm, cm, cm, -cm]
    tb = [-sm, sm, sm, sm]
    for e in range(4):
        A[64 + 16 * e:64 + 16 * e + 16] = sc * tq4[e]
        B[64 + 16 * e:64 + 16 * e + 16] = ta[e]
        Cc[64 + 16 * e:64 + 16 * e + 16] = tb[e]
    sk = np.arange(128)[:, None]
    tq_ = np.arange(128)[None, :]
    for dl in range(9):
        dist = 128 * dl + tq_ - sk
        m = ((dist >= 0) & (dist <= 128)).astype(np.float32) + ((dist >= 0) & (dist % 4 == 0) & (dist <= 512)) + \
            ((dist >= 0) & (dist % 16 == 0))
        cb[:, CB_DM + 128 * dl:CB_DM + 128 * dl + 128] = m
    cmk = np.ones((128, 512), np.float32)
    cmk[:, 0:128] = (tq_ >= sk)
    cb[:, CB_CM:CB_CM + 512] = cmk
    cb[:, CB_ID:CB_ID + 128] = np.eye(128, dtype=np.float32)
    cb[:, CB_ONE:CB_ONE + 512] = 1.0
    ind = np.zeros((8, S), np.float32)
    for n in range(8):
        ind[n, 256 * n:256 * n + 256] = 1.0
    return cb, ind


def build_small(inp):
    sm = np.zeros((128, NSM), np.float32)
    for l in range(DEPTH):
        for nm, off in (('g_pre_mix', SM_GPM), ('g_post_mix', SM_GQM), ('g_pre_mlp', SM_GPL), ('g_post_mlp', SM_GQL)):
            sm[:, off + 8 * l:off + 8 * l + 8] = inp[nm][l].reshape(8, 128).T
        sm[:, SM_GCQ + 2 * l:SM_GCQ + 2 * l + 2] = inp['g_cq'][l].reshape(2, 128).T
        sm[:, SM_GCKV + l] = inp['g_ckv'][l]
        for h in range(4):
            sm[0:4, SM_BF + 4 * l + h] = inp['b_forget'][l, h]
    aug = np.array([[-1, 0, 0, 0], [0, -1, 0, 0], [0, 0, 1, 1], [0, 0, 1, 0], [0, 0, 0, 1], [1, 1, 0, 0]], np.float32).T
    sm[0:4, SM_AUG:SM_AUG + 6] = aug
    sm[:, SM_EPS] = 1e-6
    sm[:, SM_ONE] = 1.0
    for tbi in range(8):
        b = (8 + tbi) // 2
        for n in range(8):
            sm[:, SM_NEG + 8 * tbi + n] = 0.0 if n < b else -1e30
            sm[:, SM_VAL + 8 * tbi + n] = 1.0 if n < b else 0.0
    return sm


def build_weights(inp, nlayers):
    out = np.zeros((nlayers, 128, FTOT), np.float32)
    for l in range(nlayers):
        W = {k: np.asarray(inp[k][l]) for k in ('w_in', 'w_uq', 'w_uk', 'w_uv', 'w_branch', 'w_out', 'w_up', 'w_down')}
        for k, (f, fill) in UNITS.items():
            a = fill(W)
            assert a.shape == (128, f), (k, a.shape, f)
            out[l, :, UOFF[k]:UOFF[k] + f] = a
    return out


STOP = 99
SQENG = 'pool'
EVAC_DVE = True
RS_ALT = False
PN_INTER = False
EVAC2_DVE = False
NPT = 3
MASK_ALT = False
EARLY_PN = True


def build_program(nseq, nlayers, debug=False, phases='padmfgol'):
    nc = bass.Bass("TRN2", target_bir_lowering=False)
    xin = nc.dram_tensor("xT", [nseq, 128, 8 * S], F32, kind="ExternalInput").ap()
    wts = nc.dram_tensor("wts", [nlayers, 128, FTOT], F32, kind="ExternalInput").ap()
    smd = nc.dram_tensor("small", [128, NSM], F32, kind="ExternalInput").ap()
    cbd = nc.dram_tensor("cb", [128, NCB], F32, kind="ExternalInput").ap()
    indd = nc.dram_tensor("ind", [8, S], F32, kind="ExternalInput").ap()
    yout = nc.dram_tensor("yT", [nseq, 128, 8 * S], F32, kind="ExternalOutput").ap()
    if debug:
        dbg = nc.dram_tensor("dbg", [128, 8 * S], F32, kind="ExternalOutput").ap()

    es = contextlib.ExitStack()
    with es:
        def sb(name, shape, dt):
            return es.enter_context(nc.sbuf_tensor(name, shape, dt))

        def pt(name, shape, dt):
            return es.enter_context(nc.psum_tensor(name, shape, dt))

        xT = sb("xT_sb", [128, 8 * S], F32)
        hT = sb("hT", [128, 8 * S], BF16)
        YT = sb("YT", [128, 8 * S], BF16)
        PH = sb("PH", [128, 14400], BF16)
        QKh = PH[:, 0:8192]
        Vt = PH[:, 8192:12352]
        Ytok = PH[:, 12352:14400]
        WR = sb("WR", [128, NSLOT * WSLOT], BF16)
        CB = sb("CB", [128, NCB], BF16)
        SM = sb("SM", [128, NSM], F32)
        PT_ = [sb("PT%d" % i, [128, 512], BF16) for i in range(NPT)]
        T0 = sb("T0", [128, 512], F32)
        T1 = sb("T1", [128, 512], F32)
        B0 = sb("B0", [128, 512], BF16)
        B1 = sb("B1", [128, 512], BF16)
        SMALLT = sb("SMALLT", [128, 256], F32)
        PEN = sb("PEN", [128, 64], BF16)
        KSB = sb("KSB", [128, 16], BF16)
        PENT = sb("PENT", [8, 512], BF16)
        FX = sb("FX", [4, 2 * 512], F32)
        FWT = sb("FWT", [128, 128], BF16)
        FXB = sb("FXB", [4, 512], BF16)

        PJ = [pt("PJ%d" % i, [128, 512], F32) for i in range(2)]
        STp = [pt("ST%d" % i, [128, 512], F32) for i in range(3)]
        OAp = [pt("OA%d" % i, [128, 512], F32) for i in range(2)]
        MS = pt("MS", [128, 512], F32)

        P = Prog(nc)
        cnt = {'pj': 0, 'st': 0, 'oa': 0, 'pt': 0, 'tb': 0, 'sq': 0}

        def rr(name, n):
            v = cnt[name] % n
            cnt[name] += 1
            return v

        def mm(out, lhsT, rhs, start, stop, r, w, sgc=False):
            P.op('pe', lambda e: e.matmul(out, lhsT=lhsT, rhs=rhs, start=start, stop=stop, skip_group_check=sgc), r=r, w=w)

        def act(out, in_, func, r, w, scale=None, bias=None):
            kw = {}
            if scale is not None:
                kw['scale'] = scale
            if bias is not None:
                kw['bias'] = bias
            P.op('act', lambda e: e.activation(out=out, in_=in_, func=func, **kw), r=r, w=w)

        def tt(eng, out, in0, in1, op, r, w):
            P.op(eng, lambda e: e.tensor_tensor(out=out, in0=in0, in1=in1, op=op), r=r, w=w)

        def ts(eng, out, in0, s1, op0, r, w, s2=None, op1=None):
            if op1 is None:
                P.op(eng, lambda e: e.tensor_scalar(out=out, in0=in0, scalar1=s1, scalar2=None, op0=op0), r=r, w=w)
            else:
                P.op(eng, lambda e: e.tensor_scalar(out=out, in0=in0, scalar1=s1, scalar2=s2, op0=op0, op1=op1), r=r, w=w)

        def stt(out, in0, scalar, in1, op0, op1, r, w):
            P.op('dve', lambda e: e.scalar_tensor_tensor(out=out, in0=in0, scalar=scalar, in1=in1, op0=op0, op1=op1),
                 r=r, w=w)

        def cp(eng, out, in_, r, w):
            if eng == 'act':
                P.op('act', lambda e: e.copy(out=out, in_=in_), r=r, w=w)
            else:
                P.op(eng, lambda e: e.tensor_copy(out=out, in_=in_), r=r, w=w)

        def dma(eng, out, in_, r, w):
            P.op(eng, lambda e: e.dma_start(out=out, in_=in_), r=r, w=w, dma=True)

        order_one = unit_order()
        stream = []
        for s_ in range(nseq):
            for l_ in range(nlayers):
                for u in order_one:
                    ph = {'mla': 'a', 'dil': 'd', 'mob': 'm', 'fox': 'f', 'mg_': 'g', 'wo_': 'o', 'up_': 'l', 'dn_': 'l'}[u[:3]]
                    if ph in phases:
                        stream.append((l_, u))
        wstate = {'next_load': 0, 'next_use': 0}

        def w_issue():
            i = wstate['next_load']
            if i >= len(stream):
                return
            l_, u = stream[i]
            f = UNITS[u][0]
            slot = i % NSLOT
            dma('pool', WR[:, slot * WSLOT:slot * WSLOT + f], wts[l_, :, UOFF[u]:UOFF[u] + f], r=[], w=['WR%d' % slot])
            wstate['next_load'] += 1

        def w_get(l_, u):
            i = wstate['next_use']
            assert stream[i] == (l_, u), (stream[i], l_, u)
            while wstate['next_load'] <= i:
                w_issue()
            slot = i % NSLOT
            wstate['next_use'] += 1
            return slot * WSLOT, 'WR%d' % slot

        def w_prefetch():
            while wstate['next_load'] < min(len(stream), wstate['next_use'] + NSLOT - 1):
                w_issue()

        dma('sp', SM[:, :], smd[:, :], r=[], w=['SM'])
        for k in range(0, NCB, 2048):
            k2 = min(NCB, k + 2048)
            dma('pool', CB[:, k:k2], cbd[:, k:k2], r=[], w=['CB'])
        ident = CB[:, CB_ID:CB_ID + 128]
        ones = CB[:, CB_ONE:CB_ONE + 128]
        RS = T1
        GT = SMALLT[:, 0:64]
        TOP8 = SMALLT[:, 64:128]
        RINV = SMALLT[:, 128:132]
        KSF = SMALLT[:, 132:140]
        ONES4 = CB[0:4, CB_ONE:CB_ONE + 512]
        Vv = Vt.rearrange("p (t h d) -> p t h d", t=NTB, h=4)

        def vkeys(tb):
            return ['PV%d' % k for k in range((260 * tb) // 512, (260 * tb + 259) // 512 + 1)]

        def allvkeys():
            return ['PV%d' % k for k in range(9)]

        def qkey(b, tc):
            return 'PH%d' % (8 * b + tc)

        def kkey(b, tc):
            return 'PH%d' % (8 * b + 4 + tc)

        def mxap(c):
            return hT[:, c * S + 1024:c * S + 2048].bitcast(F32)

        def mxkeys(c):
            return ['h%d_2' % c, 'h%d_3' % c]

        def mgap(hf, d, t2):
            if hf == 0:
                return PH[:, d * 1024 + t2 * 512:d * 1024 + t2 * 512 + 512]
            return hT[:, d * S + t2 * 512:d * S + t2 * 512 + 512]

        def mgkey(hf, d, t2):
            if hf == 0:
                return 'PH%d' % (2 * d + t2)
            return 'h%d_%d' % (d, t2)

        def accap(t2):
            return PH[:, 8192 + 1024 * t2:8192 + 1024 * t2 + 1024].bitcast(F32)

        def acckeys(t2):
            return ['PV%d' % (2 * t2), 'PV%d' % (2 * t2 + 1)]

        def v_set_ones():
            P.op('pool', lambda e: e.memset(Vt, 1.0), w=allvkeys())

        def xk(c, tc):
            return 'x%d_%d' % (c, tc)

        def hk(c, tc):
            return 'h%d_%d' % (c, tc)

        def yk(blk):
            return 'Y%d' % blk

        def xs(c, tc):
            return xT[:, c * S + tc * 512:c * S + tc * 512 + 512]

        def hs(c, t0, n):
            return hT[:, c * S + t0:c * S + t0 + n]

        def ys(c, t0, n):
            return YT[:, c * S + t0:c * S + t0 + n]

        rsst = {'i': 0}

        def RSc():
            return (T1 if rsst['i'] % 2 == 1 else T0)[:, :]

        def RSk():
            return 'T1' if rsst['i'] % 2 == 1 else 'T0'

        def rstd_from_ms(nfeat, rkeys):
            if RS_ALT:
                rsst['i'] += 1
            else:
                rsst['i'] = 1
            act(RSc(), MS[:, :], AF.Ln, r=['MS', 'SM'] + rkeys, w=[RSk()], scale=1.0 / nfeat, bias=SM[:, SM_EPS:SM_EPS + 1])
            act(RSc(), RSc(), AF.Exp, r=[RSk()], w=[RSk()], scale=-0.5)

        def prenorm_gen(gcol):
            for tc in range(NTC):
                prenorm(gcol, only_tc=tc, slot=tc)
                yield

        def prenorm(gcol, only_tc=None, slot=0):
            for tc in (range(NTC) if only_tc is None else [only_tc]):
                dtc = tc if only_tc is None else slot
                for c in range(8):
                    sq = B0 if (c % 2 == 0) else B1
                    sk_ = 'B0' if (c % 2 == 0) else 'B1'
                    act(sq[:, :], xs(c, tc), AF.Square, r=[xk(c, tc)], w=[sk_])
                    mm(MS[:, :], ones, sq[:, :], c == 0, c == 7, r=[sk_, 'CB'], w=['MS'])
                rstd_from_ms(1024.0, [])
                for c in range(8):
                    stt(hs(c, dtc * 512, 512), xs(c, tc), SM[:, gcol + c:gcol + c + 1], RSc(), ALU.mult, ALU.mult,
                        r=[xk(c, tc), RSk(), 'SM'], w=[hk(c, dtc)])

        def postnorm(tc, gcol):
            for c in range(8):
                sq = B0 if (c % 2 == 0) else B1
                sk_ = 'B0' if (c % 2 == 0) else 'B1'
                mx = mxap(c)
                act(sq[:, :], mx, AF.Square, r=mxkeys(c), w=[sk_])
                mm(MS[:, :], ones, sq[:, :], c == 0, c == 7, r=[sk_, 'CB'], w=['MS'])
            rstd_from_ms(1024.0, [])
            for c in range(8):
                mx = mxap(c)
                tt('pool', mx, mx, RSc(), ALU.mult, r=mxkeys(c) + [RSk()], w=mxkeys(c))
                stt(xs(c, tc), mx, SM[:, gcol + c:gcol + c + 1], xs(c, tc), ALU.mult, ALU.add,
                    r=mxkeys(c) + ['SM', xk(c, tc)], w=[xk(c, tc)])

        def qkbuf(b):
            QT = PH[:, (2 * b) * S:(2 * b + 1) * S]
            KT = PH[:, (2 * b + 1) * S:(2 * b + 2) * S]
            return QT, KT

        def attention_gen(b, R, h, dil, hcol, post=None):
            QT, KT = qkbuf(b)
            tiles = [(I, j) for I in range(4) for j in range(4 * I + 4)]
            info = {}

            def emit_qk(k):
                I, j = tiles[k]
                r0 = max(0, j - 4 * I)
                q0 = 512 * I + 128 * r0
                n = 512 - 128 * r0
                st = rr('st', 3)
                mm(STp[st][:, 0:n], KT[0:R, 128 * j:128 * j + 128], QT[0:R, q0:q0 + n], True, True,
                   r=[qkey(b, I), kkey(b, j // 4)], w=['ST%d' % st])
                info[k] = (st, r0, n)

            pinfo = {}

            def emit_exp(k):
                I, j = tiles[k]
                st, r0, n = info[k]
                ST = STp[st]
                stk = 'ST%d' % st
                pi = rr('pt', NPT)
                PTt = PT_[pi]
                ptk = 'PT%d' % pi
                act(PTt[:, 0:n], ST[:, 0:n], AF.Exp, r=[stk], w=[ptk])
                meng = 'pool' if (MASK_ALT and k % 2 == 1) else 'dve'
                if dil:
                    d0 = min(max(4 * I - j, 0), 5)
                    msk = CB[:, CB_DM + 128 * d0:CB_DM + 128 * d0 + n]
                    tt(meng, PTt[:, 0:n], PTt[:, 0:n], msk, ALU.mult, r=[ptk, 'CB'], w=[ptk])
                elif j >= 4 * I:
                    msk = CB[:, CB_CM:CB_CM + n]
                    tt(meng, PTt[:, 0:n], PTt[:, 0:n], msk, ALU.mult, r=[ptk, 'CB'], w=[ptk])
                pinfo[k] = (PTt, ptk)

            emit_qk(0)
            emit_qk(1)
            emit_exp(0)
            oa = 0
            for k, (I, j) in enumerate(tiles):
                if k + 2 < len(tiles):
                    emit_qk(k + 2)
                if k + 1 < len(tiles):
                    emit_exp(k + 1)
                if j == 0:
                    oa = rr('oa', 2)
                OA = OAp[oa]
                oak = 'OA%d' % oa
                st, r0, n = info[k]
                PTt, ptk = pinfo[k]
                for r_ in range(r0, 4):
                    i = 4 * I + r_
                    mm(OA[:, 65 * r_:65 * r_ + 65], PTt[:, 128 * (r_ - r0):128 * (r_ - r0) + 128],
                       Vv[:, j, h, :], (j == 0 and r_ == 0), j == i, r=[ptk] + vkeys(j), w=[oak], sgc=True)
                if j == 4 * I + 3:
                    OAv = OA[:, 0:260].rearrange("p (r d) -> p r d", r=4)
                    P.op('dve', lambda e, OAv=OAv: e.reciprocal(out=RINV, in_=OAv[:, :, 64]), r=[oak], w=['RINV'])
                    for r_ in range(4):
                        tb = 4 * I + r_
                        ts('dve', Ytok[:, tb * 128 + hcol:tb * 128 + hcol + 64], OA[:, 65 * r_:65 * r_ + 64],
                           RINV[:, r_:r_ + 1], ALU.mult, r=[oak, 'RINV'], w=['YK%d' % I])
                yield
            if post is not None:
                post()
                yield

        def run_jobs(jobs):
            pending = None
            for (pgi, pga, agf) in jobs:
                if pgi is not None:
                    g = pgi()
                    a_done = pending is None
                    b_done = False
                    while not (a_done and b_done):
                        if not a_done:
                            try:
                                next(pending)
                            except StopIteration:
                                a_done = True
                        if not b_done:
                            try:
                                next(g)
                            except StopIteration:
                                b_done = True
                elif pending is not None:
                    for _ in pending:
                        pass
                pending = None
                if pga is not None:
                    for _ in pga():
                        pass
                pending = agf()
            if pending is not None:
                for _ in pending:
                    pass

        def ytok_to_YT(chunk):
            for g4 in range(4):
                pj = rr('pj', 2)
                ps, pk = PJ[pj], 'PJ%d' % pj
                for k in range(4):
                    tb = 4 * g4 + k
                    mm(ps[:, 128 * k:128 * k + 128], Ytok[:, tb * 128:tb * 128 + 128], ident, True, True,
                       r=['YK%d' % g4, 'CB'], w=[pk])
                cp('act', ys(chunk, g4 * 512, 512), ps[:, :], r=[pk], w=[yk(4 * chunk + g4)])

        def v_project_gen(woff, wkey, ncols_tot):
            for tb2 in range(NTB // 2):
                pj = rr('pj', 2)
                ps = PJ[pj]
                pk = 'PJ%d' % pj
                for half in range(2):
                    tb = 2 * tb2 + half
                    for kc in range(8):
                        mm(ps[:, 256 * half:256 * half + 256], hs(kc, tb * 128, 128),
                           WR[:, woff + kc * ncols_tot:woff + kc * ncols_tot + 256], kc == 0, kc == 7,
                           r=[wkey, hk(kc, tb // 4)], w=[pk])
                cp('act', Vv[:, 2 * tb2:2 * tb2 + 2, :, 0:64],
                   ps[:, :].rearrange("p (t h d) -> p t h d", t=2, h=4), r=[pk], w=vkeys(2 * tb2) + vkeys(2 * tb2 + 1))
                yield

        hb = {'i': 0}

        def next_buf():
            b = hb['i'] % 2
            hb['i'] += 1
            return b

        def jobs_rope8(l, nm, n_mix, is_moba):
            jobs = []
            for h in range(4):
                b = next_buf()

                def pgi(h=h, b=b):
                    woff, wkey = w_get(l, '%s_h%d' % (nm, h))
                    w_prefetch()
                    QT, KT = qkbuf(b)
                    if is_moba:
                        P.op('pool', lambda e, QT=QT: e.memset(QT[96:120, 0:1024], 0.0),
                             w=[qkey(b, 0), qkey(b, 1)])
                        dma('pool', KT[112:120, :], indd[:, :], r=[], w=[kkey(b, t) for t in range(4)])
                    for which, tbl, dst, scale in (('k', CB_ROPE + 2048, KT, None), ('q', CB_ROPE, QT, 0.125)):
                        c0 = 0 if which == 'q' else 112
                        for tc in range(NTC):
                            pj = rr('pj', 2)
                            ps = PJ[pj]
                            pk = 'PJ%d' % pj
                            for kc in range(8):
                                mm(ps[0:112, :], WR[:, woff + kc * 224 + c0:woff + kc * 224 + c0 + 112], hs(kc, tc * 512, 512),
                                   kc == 0, kc == 7, r=[wkey, hk(kc, tc)], w=[pk])
                            dk = qkey(b, tc) if which == 'q' else kkey(b, tc)
                            tt('dve', dst[0:64, tc * 512:tc * 512 + 512], ps[0:64, :], CB[0:64, tbl + tc * 512:tbl + tc * 512 + 512],
                               ALU.mult, r=[pk, 'CB'], w=[dk])
                            if scale is None:
                                cp('dve' if EVAC_DVE else 'act', dst[64:112, tc * 512:tc * 512 + 512], ps[64:112, :], r=[pk], w=[dk])
                            else:
                                if EVAC_DVE:
                                    ts('dve', dst[64:112, tc * 512:tc * 512 + 512], ps[64:112, :], scale, ALU.mult, r=[pk], w=[dk])
                                else:
                                    act(dst[64:112, tc * 512:tc * 512 + 512], ps[64:112, :], AF.Copy, r=[pk], w=[dk], scale=scale)
                            yield
                    if is_moba:
                        moba_gate(b)
                        yield

                def pga():
                    woff, wkey = w_get(l, nm + '_v')
                    w_prefetch()
                    for _ in v_project_gen(woff, wkey, 256):
                        yield

                def agf(h=h, b=b):
                    post = (lambda: ytok_to_YT(2 * n_mix + h // 2)) if h % 2 == 1 else None
                    return attention_gen(b, 120 if is_moba else 112, h, nm == 'dil', (h % 2) * 64, post)

                jobs.append((pgi, pga if h == 0 else None, agf))
            return jobs

        def moba_gate(b):
            QT, KT = qkbuf(b)
            kk = [kkey(b, t) for t in range(4)]
            P.op('dve', lambda e: e.tensor_reduce(out=KSF[0:112, :], in_=KT[0:112, :].rearrange("p (n k) -> p n k", n=8),
                                                  axis=AX.X, op=ALU.add), r=kk, w=['KSF'])
            cp('dve', KSB[0:112, 0:8], KSF[0:112, :], r=['KSF'], w=['KSB'])
            tt('dve', KSB[0:112, 8:16], KSF[0:112, :], KSB[0:112, 0:8], ALU.subtract, r=['KSF', 'KSB'], w=['KSB'])
            for tbi in range(8):
                tb = 8 + tbi
                mm(MS[:, 8 * tbi:8 * tbi + 8], QT[0:112, tb * 128:tb * 128 + 128], KSB[0:112, 0:8], True, False,
                   r=['KSB', qkey(b, tb // 4)], w=['MS'])
                mm(MS[:, 8 * tbi:8 * tbi + 8], QT[0:112, tb * 128:tb * 128 + 128], KSB[0:112, 8:16], False, True,
                   r=['KSB', qkey(b, tb // 4)], w=['MS'])
            tt('dve', GT, MS[:, 0:64], SM[:, SM_NEG:SM_NEG + 64], ALU.add, r=['MS', 'SM'], w=['GT'])
            for tbi in range(8):
                P.op('dve', lambda e, tbi=tbi: e.max(out=TOP8[:, 8 * tbi:8 * tbi + 8], in_=GT[:, 8 * tbi:8 * tbi + 8]),
                     r=['GT'], w=['TOP8'])
            for tbi in range(8):
                ts('dve', PEN[:, 8 * tbi:8 * tbi + 8], GT[:, 8 * tbi:8 * tbi + 8], TOP8[:, 8 * tbi + 2:8 * tbi + 3], ALU.is_lt,
                   r=['GT', 'TOP8'], w=['PEN'], s2=-30000.0, op1=ALU.mult)
            tt('dve', PEN[:, :], PEN[:, :], SM[:, SM_VAL:SM_VAL + 64], ALU.mult, r=['PEN', 'SM'], w=['PEN'])
            for half in range(2):
                for k in range(4):
                    tbi = 4 * half + k
                    mm(MS[0:8, 128 * k:128 * k + 128], PEN[:, 8 * tbi:8 * tbi + 8], ident, True, True,
                       r=['PEN', 'CB'], w=['MS'])
                cp('dve', PENT[:, :], MS[0:8, :], r=['MS'], w=['PENT'])
                dma('sp', QT[112:120, 1024 + 512 * half:1024 + 512 * half + 512], PENT[:, :], r=['PENT'],
                    w=[qkey(b, 2 + half)])

        def jobs_fox(l):
            jobs = []
            wst = {}
            for h in range(4):
                b = next_buf()

                def pgi(h=h, b=b):
                    if h == 0:
                        wof, wkf = w_get(l, 'fox_f')
                        w_prefetch()
                        P.op('dve', lambda e: e.tensor_copy(out=FWT[:, :], in_=WR[:, wof:wof + 128]), r=[wkf], w=['FWT'])
                    woff, wkey = w_get(l, 'fox_h%d' % h)
                    w_prefetch()
                    QT, KT = qkbuf(b)
                    hoff = woff
                    prev_cs = None
                    for tc in range(NTC):
                        pj = rr('pj', 2)
                        ps = PJ[pj]
                        pk = 'PJ%d' % pj
                        for kc in range(8):
                            mm(ps[:, :], WR[:, hoff + kc * 128:hoff + kc * 128 + 128], hs(kc, tc * 512, 512), kc == 0, kc == 7,
                               r=[wkey, hk(kc, tc)], w=[pk])
                        if EVAC2_DVE:
                            ts('dve', QT[0:64, tc * 512:tc * 512 + 512], ps[0:64, :], 0.125, ALU.mult, r=[pk], w=[qkey(b, tc)])
                        else:
                            act(QT[0:64, tc * 512:tc * 512 + 512], ps[0:64, :], AF.Copy, r=[pk], w=[qkey(b, tc)], scale=0.125)
                        cp('dve', KT[0:64, tc * 512:tc * 512 + 512], ps[64:128, :], r=[pk], w=[kkey(b, tc)])
                        yield
                        pj = rr('pj', 2)
                        ps = PJ[pj]
                        pk = 'PJ%d' % pj
                        for kc in range(8):
                            mm(ps[0:4, :], FWT[:, kc * 16 + 4 * h:kc * 16 + 4 * h + 4], hs(kc, tc * 512, 512), kc == 0, kc == 7,
                               r=['FWT', hk(kc, tc)], w=[pk])
                        U_ = T0[0:4, :]
                        E_ = T1[0:4, :]
                        csb = FX[0:4, 512 * (tc % 2):512 * (tc % 2) + 512]
                        csk = 'FXc%d' % (tc % 2)
                        TMP = FXB[0:4, :]
                        ts('dve', U_, ps[0:4, :], SM[0:4, SM_BF + 4 * l + h:SM_BF + 4 * l + h + 1], ALU.add, r=[pk, 'SM'], w=['T0'])
                        act(E_, U_, AF.Exp, r=['T0'], w=['T1'], scale=-1.0)
                        act(U_, E_, AF.Ln, r=['T1', 'SM'], w=['T0'], scale=1.0, bias=SM[0:4, SM_ONE:SM_ONE + 1])
                        init = 0.0 if tc == 0 else prev_cs
                        P.op('dve', lambda e, csb=csb, U_=U_, init=init: e.tensor_tensor_scan(out=csb, data0=ONES4, data1=U_,
                                                                                             initial=init, op0=ALU.mult, op1=ALU.add),
                             r=['T0', 'CB'] + ([] if tc == 0 else ['FXc%d' % ((tc - 1) % 2)]), w=[csk])
                        prev_cs = csb[:, 511:512]
                        HI = B0[0:4, :]
                        LO = B1[0:4, :]
                        cp('dve', HI, csb, r=[csk], w=['B0'])
                        tt('dve', LO, csb, HI, ALU.subtract, r=[csk, 'B0'], w=['B1'])
                        A = SM_AUG
                        ts('dve', TMP, HI, SM[0:4, A:A + 1], ALU.mult, r=['B0', 'SM'], w=['FXt'], s2=SM[0:4, A + 2:A + 3], op1=ALU.add)
                        stt(QT[64:68, tc * 512:tc * 512 + 512], LO, SM[0:4, A + 1:A + 2], TMP, ALU.mult, ALU.add,
                            r=['B1', 'FXt', 'SM'], w=[qkey(b, tc)])
                        ts('dve', TMP, HI, SM[0:4, A + 3:A + 4], ALU.mult, r=['B0', 'SM'], w=['FXt'], s2=SM[0:4, A + 5:A + 6], op1=ALU.add)
                        stt(KT[64:68, tc * 512:tc * 512 + 512], LO, SM[0:4, A + 4:A + 5], TMP, ALU.mult, ALU.add,
                            r=['B1', 'FXt', 'SM'], w=[kkey(b, tc)])
                        yield

                def pga():
                    woff, wkey = w_get(l, 'fox_v')
                    w_prefetch()
                    for _ in v_project_gen(woff, wkey, 256):
                        yield

                def agf(h=h, b=b):
                    post = (lambda: ytok_to_YT(0 + h // 2)) if h % 2 == 1 else None
                    return attention_gen(b, 68, h, False, (h % 2) * 64, post)

                jobs.append((pgi, pga if h == 0 else None, agf))
            return jobs

        def jobs_mla(l, pn=None):
            CQ0, CKV = 0, 4
            scm = 96.0 ** -0.5
            wst = {}

            def pre():
                woff, wkey = w_get(l, 'mla_lat')
                w_prefetch()
                for tc in range(NTC):
                    if pn is not None:
                        next(pn)
                    pss = []
                    for cc in range(2):
                        pj = rr('pj', 2)
                        ps = PJ[pj]
                        pk = 'PJ%d' % pj
                        pss.append((ps, pk))
                        for kc in range(8):
                            mm(ps[:, :], WR[:, woff + kc * 384 + cc * 128:woff + kc * 384 + cc * 128 + 128], hs(kc, tc * 512, 512),
                               kc == 0, kc == 7, r=[wkey, hk(kc, tc)], w=[pk])
                        sq = B0 if cc == 0 else B1
                        sk_ = 'B0' if cc == 0 else 'B1'
                        act(sq[:, :], ps[:, :], AF.Square, r=[pk], w=[sk_])
                        mm(MS[:, :], ones, sq[:, :], cc == 0, cc == 1, r=[sk_, 'CB'], w=['MS'])
                    rstd_from_ms(256.0, [])
                    for cc in range(2):
                        ps, pk = pss[cc]
                        stt(ys(CQ0 + cc, tc * 512, 512), ps[:, :], SM[:, SM_GCQ + 2 * l + cc:SM_GCQ + 2 * l + cc + 1], RSc(),
                            ALU.mult, ALU.mult, r=[pk, RSk(), 'SM'], w=[yk(4 * (CQ0 + cc) + tc)])
                    pj = rr('pj', 2)
                    ps = PJ[pj]
                    pk = 'PJ%d' % pj
                    for kc in range(8):
                        mm(ps[:, :], WR[:, woff + kc * 384 + 256:woff + kc * 384 + 384], hs(kc, tc * 512, 512), kc == 0, kc == 7,
                           r=[wkey, hk(kc, tc)], w=[pk])
                    act(B0[:, :], ps[:, :], AF.Square, r=[pk], w=['B0'])
                    mm(MS[:, :], ones, B0[:, :], True, True, r=['B0', 'CB'], w=['MS'])
                    rstd_from_ms(128.0, [])
                    stt(ys(CKV, tc * 512, 512), ps[:, :], SM[:, SM_GCKV + l:SM_GCKV + l + 1], RSc(), ALU.mult, ALU.mult,
                        r=[pk, RSk(), 'SM'], w=[yk(4 * CKV + tc)])
                    yield
                woff, wkey = w_get(l, 'mla_kr')
                w_prefetch()
                for tc in range(NTC):
                    pj1 = rr('pj', 2)
                    p1, k1 = PJ[pj1], 'PJ%d' % pj1
                    for kc in range(8):
                        mm(p1[:, :], WR[:, woff + kc * 256:woff + kc * 256 + 128], hs(kc, tc * 512, 512), kc == 0, kc == 7,
                           r=[wkey, hk(kc, tc)], w=[k1])
                    pj2 = rr('pj', 2)
                    p2, k2 = PJ[pj2], 'PJ%d' % pj2
                    for kc in range(8):
                        mm(p2[:, :], WR[:, woff + kc * 256 + 128:woff + kc * 256 + 256], hs(kc, tc * 512, 512), kc == 0, kc == 7,
                           r=[wkey, hk(kc, tc)], w=[k2])
                    ta = CB[64:128, CB_ROPE + 2048 + tc * 512:CB_ROPE + 2048 + tc * 512 + 512]
                    tb_ = CB[64:128, CB_ROPE + 4096 + tc * 512:CB_ROPE + 4096 + tc * 512 + 512]
                    tt('dve', T0[64:128, :], p2[64:128, :], tb_, ALU.mult, r=[k2, 'CB'], w=['T0'])
                    tt('dve', T1[64:128, :], p1[64:128, :], ta, ALU.mult, r=[k1, 'CB'], w=['T1'])
                    tt('dve', CB[0:64, CB_ROPE + 4096 + tc * 512:CB_ROPE + 4096 + tc * 512 + 512], T0[64:128, :], T1[64:128, :],
                       ALU.add, r=['T0', 'T1'], w=['KR%d' % tc])
                    yield
                wst['w'] = w_get(l, 'mla_up')
                w_prefetch()
                woff, wkey = wst['w']
                for tb2 in range(NTB // 2):
                    pj = rr('pj', 2)
                    ps = PJ[pj]
                    pk = 'PJ%d' % pj
                    for half in range(2):
                        tb = 2 * tb2 + half
                        mm(ps[:, 256 * half:256 * half + 256], ys(CKV, tb * 128, 128), WR[:, woff + 256:woff + 512], True, True,
                           r=[wkey, yk(4 * CKV + tb // 4)], w=[pk])
                    cp('act', Vv[:, 2 * tb2:2 * tb2 + 2, :, 0:64], ps[:, :].rearrange("p (t h d) -> p t h d", t=2, h=4),
                       r=[pk], w=vkeys(2 * tb2) + vkeys(2 * tb2 + 1))
                    yield

            jobs = []
            for h in range(4):
                b = next_buf()

                def pgi(h=h, b=b):
                    woff, wkey = wst['w']
                    QT, KT = qkbuf(b)
                    for tc in range(NTC):
                        pj = rr('pj', 2)
                        ps, pk = PJ[pj], 'PJ%d' % pj
                        mm(ps[0:64, :], WR[:, woff + 64 * h:woff + 64 * h + 64], ys(CKV, tc * 512, 512), True, True,
                           r=[wkey, yk(4 * CKV + tc)], w=[pk])
                        cp('dve' if EVAC2_DVE else 'act', KT[0:64, tc * 512:tc * 512 + 512], ps[0:64, :], r=[pk], w=[kkey(b, tc)])
                        cp('dve', KT[64:128, tc * 512:tc * 512 + 512],
                           CB[0:64, CB_ROPE + 4096 + tc * 512:CB_ROPE + 4096 + tc * 512 + 512], r=['KR%d' % tc], w=[kkey(b, tc)])
                        pj = rr('pj', 2)
                        ps, pk = PJ[pj], 'PJ%d' % pj
                        for kc in range(2):
                            mm(ps[:, :], WR[:, woff + 512 + kc * 512 + 128 * h:woff + 512 + kc * 512 + 128 * h + 128],
                               ys(CQ0 + kc, tc * 512, 512), kc == 0, kc == 1, r=[wkey, yk(4 * (CQ0 + kc) + tc)], w=[pk])
                        if EVAC2_DVE:
                            ts('dve', QT[0:64, tc * 512:tc * 512 + 512], ps[0:64, :], scm, ALU.mult, r=[pk], w=[qkey(b, tc)])
                        else:
                            act(QT[0:64, tc * 512:tc * 512 + 512], ps[0:64, :], AF.Copy, r=[pk], w=[qkey(b, tc)], scale=scm)
                        tt('dve', QT[64:128, tc * 512:tc * 512 + 512], ps[64:128, :],
                           CB[64:128, CB_ROPE + tc * 512:CB_ROPE + tc * 512 + 512], ALU.mult, r=[pk, 'CB'], w=[qkey(b, tc)])
                        yield

                def agf(h=h, b=b):
                    post = (lambda: ytok_to_YT(6 + h // 2)) if h % 2 == 1 else None
                    return attention_gen(b, 128, h, False, (h % 2) * 64, post)

                if h == 0:
                    def pga0(pgi=pgi):
                        for _ in pre():
                            yield
                        for _ in pgi():
                            yield
                    jobs.append((None, pga0, agf))
                else:
                    jobs.append((pgi, None, agf))
            return jobs

        def merge(l, hf):
            for d in range(8):
                for npair in range(2):
                    woff, wkey = w_get(l, 'mg_%d_%d' % (d, npair))
                    w_prefetch()
                    for nn in range(2):
                        n = 2 * npair + nn
                        go = woff + nn * 1280
                        bo = go + 1024
                        for t2 in range(2):
                            tc = 2 * hf + t2
                            pj = rr('pj', 2)
                            pg, kg = PJ[pj], 'PJ%d' % pj
                            for kc in range(8):
                                mm(pg[:, :], WR[:, go + kc * 128:go + kc * 128 + 128], hs(kc, tc * 512, 512), kc == 0, kc == 7,
                                   r=[wkey, hk(kc, tc)], w=[kg])
                            st = rr('st', 3)
                            pu, ku = STp[st], 'ST%d' % st
                            for kc in range(2):
                                mm(pu[:, :], WR[:, bo + kc * 128:bo + kc * 128 + 128], ys(2 * n + kc, tc * 512, 512), kc == 0, kc == 1,
                                   r=[wkey, yk(4 * (2 * n + kc) + tc)], w=[ku])
                            sg, sgk = (T0, 'T0') if (t2 == 0) else (T1, 'T1')
                            act(sg[:, :], pg[:, :], AF.Sigmoid, r=[kg], w=[sgk])
                            acc = accap(t2)
                            acck = acckeys(t2)
                            if n == 0:
                                tt('dve', acc, pu[:, :], sg[:, :], ALU.mult, r=[ku, sgk], w=acck)
                            else:
                                tt('dve', sg[:, :], pu[:, :], sg[:, :], ALU.mult, r=[ku, sgk], w=[sgk])
                                tt('pool', acc, acc, sg[:, :], ALU.add, r=acck + [sgk], w=acck)
                            if n == 3:
                                cp('act', mgap(hf, d, t2), acc, r=acck, w=[mgkey(hf, d, t2)])

        def wout(l):
            wo = []
            for u in range(2):
                wo.append(w_get(l, 'wo_%d' % u))
            for hf in range(2):
                for t2 in range(2):
                    tc = 2 * hf + t2
                    for dc in range(8):
                        woff, wkey = wo[dc // 4]
                        pj = rr('pj', 2)
                        ps, pk = PJ[pj], 'PJ%d' % pj
                        for kc in range(8):
                            mm(ps[:, :], WR[:, woff + kc * 512 + (dc % 4) * 128:woff + kc * 512 + (dc % 4) * 128 + 128],
                               mgap(hf, kc, t2), kc == 0, kc == 7, r=[wkey, mgkey(hf, kc, t2)], w=[pk])
                        cp('act', mxap(dc), ps[:, :], r=[pk], w=mxkeys(dc))
                    postnorm(tc, SM_GQM + 8 * l)
            w_prefetch()

        def mlp(l):
            gpre = SM_GPL + 8 * l
            gpost = SM_GQL + 8 * l

            def h2ap(kc, sub):
                return PH[:, kc * 1024 + sub * 512:kc * 1024 + sub * 512 + 512]

            def h2key(kc, sub):
                return 'PH%d' % (2 * kc + sub)

            def mx2(dc, sub):
                return hT[:, dc * S + sub * 1024:dc * S + sub * 1024 + 1024].bitcast(F32)

            def mx2keys(dc, sub):
                return [hk(dc, 2 * sub), hk(dc, 2 * sub + 1)]

            for th in range(2):
                for sub in range(2):
                    tc = 2 * th + sub
                    for c in range(8):
                        sq = B0 if (c % 2 == 0) else B1
                        sk_ = 'B0' if (c % 2 == 0) else 'B1'
                        act(sq[:, :], xs(c, tc), AF.Square, r=[xk(c, tc)], w=[sk_])
                        mm(MS[:, :], ones, sq[:, :], c == 0, c == 7, r=[sk_, 'CB'], w=['MS'])
                    rstd_from_ms(1024.0, [])
                    for c in range(8):
                        stt(h2ap(c, sub), xs(c, tc), SM[:, gpre + c:gpre + c + 1], RSc(), ALU.mult, ALU.mult,
                            r=[xk(c, tc), RSk(), 'SM'], w=[h2key(c, sub)])
                for fh in range(2):
                    for u in range(4 * fh, 4 * fh + 4):
                        woff, wkey = w_get(l, 'up_%d' % u)
                        w_prefetch()
                        for fq in range(4):
                            fcl = 4 * (u - 4 * fh) + fq
                            for sub in range(2):
                                pj = rr('pj', 2)
                                ps, pk = PJ[pj], 'PJ%d' % pj
                                for kc in range(8):
                                    mm(ps[:, :], WR[:, woff + kc * 512 + fq * 128:woff + kc * 512 + fq * 128 + 128], h2ap(kc, sub),
                                       kc == 0, kc == 7, r=[wkey, h2key(kc, sub)], w=[pk])
                                rl, rk = (B0, 'B0') if (sub == 0) else (B1, 'B1')
                                act(rl[:, :], ps[:, :], AF.Relu, r=[pk], w=[rk])
                                blk = 2 * fcl + sub
                                tt(SQENG, YT[:, blk * 512:blk * 512 + 512], rl[:, :], rl[:, :], ALU.mult, r=[rk], w=[yk(blk)])
                    for q in range(4):
                        woff, wkey = w_get(l, 'dn_%d_%d' % (fh, q))
                        w_prefetch()
                        for dq in range(2):
                            dc = 2 * q + dq
                            for sub in range(2):
                                pj = rr('pj', 2)
                                ps, pk = PJ[pj], 'PJ%d' % pj
                                for fcl in range(16):
                                    blk = 2 * fcl + sub
                                    mm(ps[:, :], WR[:, woff + fcl * 256 + dq * 128:woff + fcl * 256 + dq * 128 + 128],
                                       YT[:, blk * 512:blk * 512 + 512], fcl == 0, fcl == 15, r=[wkey, yk(blk)], w=[pk])
                                if fh == 0:
                                    cp('act', mx2(dc, sub), ps[:, :], r=[pk], w=mx2keys(dc, sub))
                                else:
                                    tt('dve', mx2(dc, sub), ps[:, :], mx2(dc, sub), ALU.add, r=[pk] + mx2keys(dc, sub),
                                       w=mx2keys(dc, sub))
                for sub in range(2):
                    tc = 2 * th + sub
                    for c in range(8):
                        sq = B0 if (c % 2 == 0) else B1
                        sk_ = 'B0' if (c % 2 == 0) else 'B1'
                        mx = mx2(c, sub)
                        act(sq[:, :], mx, AF.Square, r=mx2keys(c, sub), w=[sk_])
                        mm(MS[:, :], ones, sq[:, :], c == 0, c == 7, r=[sk_, 'CB'], w=['MS'])
                    rstd_from_ms(1024.0, [])
                    for c in range(8):
                        mx = mx2(c, sub)
                        tt('pool', mx, mx, RSc(), ALU.mult, r=mx2keys(c, sub) + [RSk()], w=mx2keys(c, sub))
                        stt(xs(c, tc), mx, SM[:, gpost + c:gpost + c + 1], xs(c, tc), ALU.mult, ALU.add,
                            r=mx2keys(c, sub) + ['SM', xk(c, tc)], w=[xk(c, tc)])

        for s_ in range(nseq):
            for c in range(8):
                dma('sp', xT[:, c * S:(c + 1) * S], xin[s_, :, c * S:(c + 1) * S], r=[], w=[xk(c, t) for t in range(4)])
            for l in range(nlayers):
                pn = None
                if 'p' in phases:
                    if PN_INTER and 'a' in phases:
                        pn = prenorm_gen(SM_GPM + 8 * l)
                    else:
                        prenorm(SM_GPM + 8 * l)
                v_set_ones()
                jobs = []
                if 'a' in phases:
                    jobs += jobs_mla(l, pn)
                if 'd' in phases:
                    jobs += jobs_rope8(l, 'dil', 1, False)
                if 'm' in phases:
                    jobs += jobs_rope8(l, 'mob', 2, True)
                if 'f' in phases:
                    jobs += jobs_fox(l)
                run_jobs(jobs)
                if debug and s_ == 0 and l == 0:
                    for c in range(8):
                        if phases == 'p':
                            dma('pool', dbg[:, c * S:(c + 1) * S], hT[:, c * S:(c + 1) * S], r=[hk(c, t) for t in range(4)], w=[])
                        else:
                            dma('pool', dbg[:, c * S:(c + 1) * S], YT[:, c * S:(c + 1) * S], r=[yk(4 * c + t) for t in range(4)], w=[])
                if 'g' in phases:
                    merge(l, 0)
                    merge(l, 1)
                if 'o' in phases:
                    wout(l)
                if 'l' in phases:
                    mlp(l)
            for c in range(8):
                dma('sp', yout[s_, :, c * S:(c + 1) * S], xT[:, c * S:(c + 1) * S], r=[xk(c, t) for t in range(4)], w=[])
        P.emit()
    return nc


def _to_fm(xs):
    n = xs.shape[0]
    return np.ascontiguousarray(xs.transpose(0, 2, 1).reshape(n, 8, 128, S).transpose(0, 2, 1, 3)).reshape(n, 128, 8 * S)


def _from_fm(yT):
    n = yT.shape[0]
    return yT.reshape(n, 128, 8, S).transpose(0, 2, 1, 3).reshape(n, D, S).transpose(0, 2, 1)


def kernel(**inputs):
    inp = {k: np.asarray(v) for k, v in inputs.items()}
    x = inp['x'].astype(np.float32, copy=False)
    nseq = x.shape[0] // NCORE
    wts = build_weights(inp, DEPTH)
    sm = build_small(inp)
    cb, ind = build_consts()
    nc = build_program(nseq, DEPTH)
    in_maps = []
    for c in range(NCORE):
        in_maps.append({'xT': _to_fm(x[c * nseq:(c + 1) * nseq]), 'wts': wts, 'small': sm, 'cb': cb, 'ind': ind})
    res = run_bass_kernel_spmd(nc, in_maps, core_ids=list(range(NCORE)))
    out = np.empty_like(x)
    for c in range(NCORE):
        out[c * nseq:(c + 1) * nseq] = _from_fm(np.asarray(res.results[c]['yT']))
    return out
```
